# Optimizing a Trainium2 kernel written in Bass

```python
import math
import jax, jax.numpy as jnp
from jax import lax
import numpy as np

D_MODEL = 2048
BATCH = 1
SEQ = 16384
DEPTH = 2

MIX_WIDTH = D_MODEL
DIFF_HEADS = 8
DIFF_DIM = 64
DIFF_WIDTH = DIFF_HEADS * 2 * DIFF_DIM
DIL_HEADS = 8
DIL_DIM = 128
DIL_WIDTH = DIL_HEADS * DIL_DIM
DIL_BRANCHES = ((128, 1), (512, 4), (2048, 16))
QKV_WIDTH = 3 * DIFF_WIDTH + 3 * DIL_WIDTH
ROPE_THETA = 10000.0
Q_BLOCK = 128
D_FF = 5632
N_EXPERTS = 8
TOP_K = 2
D_EXPERT = 7168
MOE_BLOCK = 256
EPS = 1e-6
NEG = -1e30

kernel_name = "hymba_diffattn_dilated_moe_encoder"


def rms_norm(x, g):
    xf = x.astype(jnp.float32)
    y = xf * lax.rsqrt(jnp.mean(xf * xf, axis=-1, keepdims=True) + EPS)
    return (y * g.astype(jnp.float32)).astype(x.dtype)


def rope_tables(seq, dim):
    pos = jnp.arange(seq, dtype=jnp.float32)
    inv = ROPE_THETA ** (-jnp.arange(0, dim, 2, dtype=jnp.float32) / dim)
    ang = pos[:, None] * inv[None, :]
    ang = jnp.concatenate([ang, ang], axis=-1)
    return jnp.cos(ang), jnp.sin(ang)


def apply_rope(x, cos, sin):
    xf = x.astype(jnp.float32)
    x1, x2 = jnp.split(xf, 2, axis=-1)
    rot = jnp.concatenate([-x2, x1], axis=-1)
    return (xf * cos + rot * sin).astype(x.dtype)


def diff_attention(q, k, v, lam, lam_init, q_g, k_g, o_g, cos, sin):
    c, s_ = cos[:, None, None, :], sin[:, None, None, :]
    q = apply_rope(rms_norm(q, q_g), c, s_) * (DIFF_DIM ** -0.5)
    k = apply_rope(rms_norm(k, k_g), c, s_)
    q = q.transpose(0, 2, 3, 1, 4)
    k = k.transpose(0, 2, 3, 1, 4)
    v = v.transpose(0, 2, 1, 3)
    B, H, _, S, Dd = q.shape
    nq = S // Q_BLOCK
    qb = jnp.moveaxis(q.reshape(B, H, 2, nq, Q_BLOCK, Dd), 3, 0)

    def block(qi):
        sc = jnp.einsum('bhmqd,bhmkd->bhmqk', qi, k).astype(jnp.float32)
        p = jax.nn.softmax(sc, axis=-1)
        a = p[:, :, 0] - lam * p[:, :, 1]
        return jnp.einsum('bhqk,bhkd->bhqd', a.astype(v.dtype), v)

    o = lax.map(block, qb)
    o = jnp.moveaxis(o, 0, 2).reshape(B, H, S, 2 * Dd)
    o = rms_norm(o, o_g) * (1.0 - lam_init)
    return o.transpose(0, 2, 1, 3).reshape(B, S, H * 2 * Dd)


def dilated_branch(q, k, v, window, dil):
    n_side = window // (2 * dil)
    blk = n_side
    B, H, S, D = q.shape
    L = S // dil
    nb = -(-L // blk)
    Lp = nb * blk

    def strided(t):
        return t.reshape(B, H, L, dil, D).transpose(0, 1, 3, 2, 4)

    qs = jnp.pad(strided(q), ((0, 0), (0, 0), (0, 0), (0, Lp - L), (0, 0)))
    ks = jnp.pad(strided(k), ((0, 0), (0, 0), (0, 0), (blk, Lp - L + blk), (0, 0)))
    vs = jnp.pad(strided(v), ((0, 0), (0, 0), (0, 0), (blk, Lp - L + blk), (0, 0)))
    qb = qs.reshape(B, H, dil, nb, blk, D)
    kidx = jnp.arange(nb)[:, None] * blk + jnp.arange(3 * blk)[None, :]
    kb = ks[:, :, :, kidx, :]
    vb = vs[:, :, :, kidx, :]
    m_pos = jnp.arange(nb)[:, None] * blk + jnp.arange(blk)[None, :]
    n_pos = kidx - blk
    valid = ((n_pos[:, None, :] >= 0) & (n_pos[:, None, :] < L)
             & (jnp.abs(m_pos[:, :, None] - n_pos[:, None, :]) <= n_side))
    sc = jnp.einsum('bhrnqd,bhrnkd->bhrnqk', qb, kb).astype(jnp.float32)
    sc = jnp.where(valid, sc, NEG)
    mx = jnp.max(sc, axis=-1, keepdims=True)
    p = jnp.exp(sc - mx)
    den = jnp.sum(p, axis=-1, keepdims=True)
    o = jnp.einsum('bhrnqk,bhrnkd->bhrnqd', (p / den).astype(v.dtype), vb)
    lse = (mx + jnp.log(den))[..., 0]
    o = o.reshape(B, H, dil, Lp, D)[:, :, :, :L].transpose(0, 1, 3, 2, 4).reshape(B, H, S, D)
    lse = lse.reshape(B, H, dil, Lp)[..., :L].transpose(0, 1, 3, 2).reshape(B, H, S)
    return o, lse


def dilated_attention(q, k, v, q_g, k_g, o_g, cos, sin):
    c, s_ = cos[:, None, :], sin[:, None, :]
    q = apply_rope(rms_norm(q, q_g), c, s_) * (DIL_DIM ** -0.5)
    k = apply_rope(rms_norm(k, k_g), c, s_)
    q, k, v = (t.transpose(0, 2, 1, 3) for t in (q, k, v))
    outs, lses = [], []
    for window, dil in DIL_BRANCHES:
        o, l = dilated_branch(q, k, v, window, dil)
        outs.append(o)
        lses.append(l)
    w = jax.nn.softmax(jnp.stack(lses), axis=0)
    o = jnp.einsum('gbhs,gbhsd->bhsd', w.astype(v.dtype), jnp.stack(outs))
    o = rms_norm(o, o_g)
    B, H, S, D = o.shape
    return o.transpose(0, 2, 1, 3).reshape(B, S, H * D)


def dense_swiglu(h, wg, wu, wd):
    return (jax.nn.silu(h @ wg) * (h @ wu)) @ wd


def moe_swiglu(h, router, wg, wu, wd):
    B, S, D = h.shape
    T = B * S
    xt = h.reshape(T, D)
    logits = xt.astype(jnp.float32) @ router.astype(jnp.float32)
    top_val, top_idx = lax.top_k(logits, TOP_K)
    gates = jax.nn.softmax(top_val, axis=-1)
    n_assign = T * TOP_K
    e_flat = top_idx.reshape(-1).astype(jnp.int32)
    tok_flat = jnp.repeat(jnp.arange(T, dtype=jnp.int32), TOP_K)
    g_flat = gates.reshape(-1)
    order = jnp.argsort(e_flat)
    e_s, tok_s, g_s = e_flat[order], tok_flat[order], g_flat[order]
    counts = jnp.bincount(e_flat, length=N_EXPERTS)
    starts = jnp.cumsum(counts) - counts
    nblk_e = (counts + MOE_BLOCK - 1) // MOE_BLOCK
    cum_blk = jnp.cumsum(nblk_e)
    pad_starts = (cum_blk - nblk_e) * MOE_BLOCK
    pos = pad_starts[e_s] + jnp.arange(n_assign, dtype=jnp.int32) - starts[e_s]
    n_blocks = -(-n_assign // MOE_BLOCK) + N_EXPERTS
    tok_buf = jnp.full((n_blocks * MOE_BLOCK,), T, jnp.int32).at[pos].set(tok_s)
    g_buf = jnp.zeros((n_blocks * MOE_BLOCK,), jnp.float32).at[pos].set(g_s)
    blk_expert = jnp.minimum(jnp.searchsorted(cum_blk, jnp.arange(n_blocks), side='right'),
                             N_EXPERTS - 1)
    x_pad = jnp.concatenate([xt, jnp.zeros((1, D), xt.dtype)], axis=0)

    def run(args):
        tok, e = args
        xb = x_pad[tok]
        return (jax.nn.silu(xb @ wg[e]) * (xb @ wu[e])) @ wd[e]

    ys = lax.map(run, (tok_buf.reshape(n_blocks, MOE_BLOCK), blk_expert))
    y = jnp.zeros((T + 1, D), h.dtype).at[tok_buf].add(
        ys.reshape(-1, D) * g_buf[:, None].astype(h.dtype))
    return y[:T].reshape(B, S, D)


def setup_inputs(seed: int = 0) -> dict:
    key = jax.random.key(seed)
    keys = iter(jax.random.split(key, 64))

    def nrm(shape, scale):
        return jax.random.normal(next(keys), shape, jnp.float32) * scale

    def gain(n):
        return 1.0 + nrm((n,), 0.02)

    d = {"x": nrm((BATCH, SEQ, D_MODEL), 1.0)}
    for i in range(DEPTH):
        d[f"attn_norm_{i}"] = gain(D_MODEL)
        d[f"w_in_{i}"] = nrm((D_MODEL, QKV_WIDTH), D_MODEL ** -0.5)
        d[f"diff_q_norm_{i}"] = gain(DIFF_DIM)
        d[f"diff_k_norm_{i}"] = gain(DIFF_DIM)
        d[f"diff_lam_q1_{i}"] = nrm((DIFF_DIM,), 0.1)
        d[f"diff_lam_k1_{i}"] = nrm((DIFF_DIM,), 0.1)
        d[f"diff_lam_q2_{i}"] = nrm((DIFF_DIM,), 0.1)
        d[f"diff_lam_k2_{i}"] = nrm((DIFF_DIM,), 0.1)
        d[f"diff_out_norm_{i}"] = gain(2 * DIFF_DIM)
        d[f"dil_q_norm_{i}"] = gain(DIL_DIM)
        d[f"dil_k_norm_{i}"] = gain(DIL_DIM)
        d[f"dil_out_norm_{i}"] = gain(DIL_DIM)
        d[f"w_out_{i}"] = nrm((MIX_WIDTH, D_MODEL), MIX_WIDTH ** -0.5)
        d[f"ffn_norm_{i}"] = gain(D_MODEL)
        if i % 2 == 0:
            d[f"ffn_w_gate_{i}"] = nrm((D_MODEL, D_FF), D_MODEL ** -0.5)
            d[f"ffn_w_up_{i}"] = nrm((D_MODEL, D_FF), D_MODEL ** -0.5)
            d[f"ffn_w_down_{i}"] = nrm((D_FF, D_MODEL), D_FF ** -0.5)
        else:
            d[f"router_{i}"] = nrm((D_MODEL, N_EXPERTS), D_MODEL ** -0.5)
            d[f"moe_w_gate_{i}"] = nrm((N_EXPERTS, D_MODEL, D_EXPERT), D_MODEL ** -0.5)
            d[f"moe_w_up_{i}"] = nrm((N_EXPERTS, D_MODEL, D_EXPERT), D_MODEL ** -0.5)
            d[f"moe_w_down_{i}"] = nrm((N_EXPERTS, D_EXPERT, D_MODEL), D_EXPERT ** -0.5)
    return d


def reference(x,
              attn_norm_0, w_in_0, diff_q_norm_0, diff_k_norm_0, diff_lam_q1_0, diff_lam_k1_0,
              diff_lam_q2_0, diff_lam_k2_0, diff_out_norm_0, dil_q_norm_0, dil_k_norm_0,
              dil_out_norm_0, w_out_0, ffn_norm_0, ffn_w_gate_0, ffn_w_up_0, ffn_w_down_0,
              attn_norm_1, w_in_1, diff_q_norm_1, diff_k_norm_1, diff_lam_q1_1, diff_lam_k1_1,
              diff_lam_q2_1, diff_lam_k2_1, diff_out_norm_1, dil_q_norm_1, dil_k_norm_1,
              dil_out_norm_1, w_out_1, ffn_norm_1, router_1, moe_w_gate_1, moe_w_up_1,
              moe_w_down_1):
    mix_params = [
        (attn_norm_0, w_in_0, diff_q_norm_0, diff_k_norm_0, diff_lam_q1_0, diff_lam_k1_0,
         diff_lam_q2_0, diff_lam_k2_0, diff_out_norm_0, dil_q_norm_0, dil_k_norm_0,
         dil_out_norm_0, w_out_0, ffn_norm_0),
        (attn_norm_1, w_in_1, diff_q_norm_1, diff_k_norm_1, diff_lam_q1_1, diff_lam_k1_1,
         diff_lam_q2_1, diff_lam_k2_1, diff_out_norm_1, dil_q_norm_1, dil_k_norm_1,
         dil_out_norm_1, w_out_1, ffn_norm_1),
    ]
    ffn_params = [(ffn_w_gate_0, ffn_w_up_0, ffn_w_down_0),
                  (router_1, moe_w_gate_1, moe_w_up_1, moe_w_down_1)]
    B, S, _ = x.shape
    cos_a, sin_a = rope_tables(S, DIFF_DIM)
    cos_b, sin_b = rope_tables(S, DIL_DIM)
    split_at = [int(v) for v in np.cumsum([DIFF_WIDTH] * 3 + [DIL_WIDTH] * 3)[:-1]]
    for i in range(DEPTH):
        (a_norm, w_in, dqn, dkn, lq1, lk1, lq2, lk2, don, bqn, bkn, bon, w_out,
         f_norm) = mix_params[i]
        lam_init = 0.8 - 0.6 * math.exp(-0.3 * i)
        lam = (jnp.exp(jnp.sum(lq1.astype(jnp.float32) * lk1.astype(jnp.float32)))
               - jnp.exp(jnp.sum(lq2.astype(jnp.float32) * lk2.astype(jnp.float32)))
               + lam_init)
        h = rms_norm(x, a_norm)
        proj = h @ w_in
        qa, ka, va, qb, kb, vb = jnp.split(proj, split_at, axis=-1)
        out_a = diff_attention(qa.reshape(B, S, DIFF_HEADS, 2, DIFF_DIM),
                               ka.reshape(B, S, DIFF_HEADS, 2, DIFF_DIM),
                               va.reshape(B, S, DIFF_HEADS, 2 * DIFF_DIM),
                               lam, lam_init, dqn, dkn, don, cos_a, sin_a)
        out_b = dilated_attention(qb.reshape(B, S, DIL_HEADS, DIL_DIM),
                                  kb.reshape(B, S, DIL_HEADS, DIL_DIM),
                                  vb.reshape(B, S, DIL_HEADS, DIL_DIM),
                                  bqn, bkn, bon, cos_b, sin_b)
        x = x + jnp.concatenate([out_a, out_b], axis=-1) @ w_out
        h = rms_norm(x, f_norm)
        if i % 2 == 0:
            x = x + dense_swiglu(h, *ffn_params[i])
        else:
            x = x + moe_swiglu(h, *ffn_params[i])
    return x
```

```python
import math
from contextlib import ExitStack

import numpy as np
import ml_dtypes
import concourse.bass as bass
import concourse.mybir as mybir
from concourse.bass_utils import run_bass_kernel_spmd

F32 = mybir.dt.float32
BF16 = mybir.dt.bfloat16
ALU = mybir.AluOpType
AF = mybir.ActivationFunctionType
AX = mybir.AxisListType

NCORES = 8
DM = 2048
KC = DM // 128
D_FF = 5632
D_EXP = 7168
NEXP = 8
EPS = 1e-6
ROPE_THETA = 10000.0
RG = [list(range(NCORES))]


class Prod:
    def __init__(self, sem, name):
        self.sem = sem
        self.n = 0
        self.name = name


class Buf:
    def __init__(self, t, name, partial=False):
        self.t = t
        self.name = name
        self.partial = partial
        self.w = {}
        self.r = {}
        self.ds = None

    def __getitem__(self, idx):
        return self.t[idx]


class Eng:
    def __init__(self, eng, prod):
        self.eng = eng
        self.prod = prod
        self.seen = {}


class K:
    def __init__(self, S, debug=False):
        self.S = S
        self.TB = S // NCORES
        self.NSB = self.TB // 512
        self.debug = debug
        self.nc = bass.Bass("TRN2", target_bir_lowering=False)
        self.st = ExitStack()
        self.nsem = 0
        self.dbg_out = {}
        self._all_bufs = []
        self.free_ds = []

    def sem(self, name):
        self.nsem += 1
        return self.st.enter_context(self.nc.semaphore(name))

    def mk_engines(self):
        nc = self.nc
        self.pe = Eng(nc.tensor, Prod(self.sem("s_pe"), "pe"))
        self.act = Eng(nc.scalar, Prod(self.sem("s_act"), "act"))
        self.dve = Eng(nc.vector, Prod(self.sem("s_dve"), "dve"))
        self.sp = Eng(nc.sync, Prod(None, "sp"))
        self.pool = Eng(nc.gpsimd, Prod(self.sem("s_pool"), "pool"))
        self.pid_sp = nc.sync.partition_id()
        self.pid_pool = nc.gpsimd.partition_id()

    def mkbuf(self, t, name, partial=False):
        b = Buf(t, name, partial)
        self._all_bufs.append(b)
        return b

    def _recycle(self, b):
        if b.ds is not None:
            b.ds.floor = b.ds.n
            self.free_ds.append(b.ds)
            b.ds = None

    def sb(self, name, shape, dt, stack=None, partial=False):
        st = stack if stack is not None else self.st
        self.uid = getattr(self, "uid", 0) + 1
        t = st.enter_context(self.nc.sbuf_tensor(f"{name}_{self.uid}", list(shape), dt))
        b = self.mkbuf(t, name, partial)
        b.is_sb = True
        if stack is not None:
            stack.callback(self._recycle, b)
        return b

    def ps(self, name, stack, shape=(128, 512), dt=F32):
        self.uid = getattr(self, "uid", 0) + 1
        full = [128, 512] if dt == F32 else [128, 1024]
        t = stack.enter_context(self.nc.psum_tensor(f"{name}_{self.uid}", full, dt))
        shape = tuple(shape)
        if len(shape) == 2:
            view = t[0:shape[0], 0:shape[1]]
        else:
            view = t[0:shape[0], 0:shape[1] * shape[2]].rearrange("p (a b) -> p a b", a=shape[1])
        return self.mkbuf(view, name)

    def ps_split(self, name, stack, n, w, dt):
        return [self.ps(f"{name}{j}", stack, (128, w), dt) for j in range(n)]

    def dram(self, name, shape, dt, kind="Internal"):
        t = self.nc.dram_tensor(name, list(shape), dt, kind=kind)
        return self.mkbuf(t.ap(), name, partial=True)

    def scope(self):
        st = ExitStack()
        st.callback(self.barrier)
        return st

    def barrier(self):
        prods = [self.pe.prod, self.act.prod, self.dve.prod]
        seen_ids = set()
        for b in self._all_bufs:
            if getattr(b, "is_sb", False) and b.ds is not None and id(b.ds) not in seen_ids:
                seen_ids.add(id(b.ds))
                prods.append(b.ds)
        for p in self.free_ds:
            if id(p) not in seen_ids:
                seen_ids.add(id(p))
                prods.append(p)
        for E in (self.pe, self.act, self.dve, self.sp):
            for p in prods:
                if p is not E.prod and p.n > 0 and E.seen.get(p, 0) < p.n:
                    E.eng.wait_ge(p.sem, p.n)
                    E.seen[p] = p.n

    def get_ds(self, sig, pre, fresh=False):
        if sig.ds is None:
            if self.free_ds and not fresh:
                sig.ds = self.free_ds.pop()
            else:
                sig.ds = Prod(self.sem(pre + sig.name), pre + sig.name)
                sig.ds.floor = 0
        return sig.ds

    def finish_all(self):
        nc = self.nc
        prods = set()
        for b in self._all_bufs:
            if b.ds is not None:
                prods.add(b.ds)
        for p in list(prods) + self.free_ds + [self.pe.prod, self.act.prod, self.dve.prod]:
            if p.n > 0 and self.sp.seen.get(p, 0) < p.n:
                nc.sync.wait_ge(p.sem, p.n)
                self.sp.seen[p] = p.n

    def _waits(self, E, reads, writes, ss=False):
        need = {}
        for b in reads:
            for p, c in b.w.items():
                if need.get(p, 0) < c:
                    need[p] = c
        for b in writes:
            if not b.partial:
                for p, c in b.w.items():
                    if need.get(p, 0) < c:
                        need[p] = c
            for p, c in b.r.items():
                if need.get(p, 0) < c:
                    need[p] = c
        for p, c in need.items():
            if p is E.prod and not ss:
                continue
            if E.seen.get(p, 0) < c:
                E.eng.wait_ge(p.sem, c)
                E.seen[p] = c

    def _record(self, prod, cnt, reads, writes):
        for b in reads:
            if b.r.get(prod, 0) < cnt:
                b.r[prod] = cnt
        for b in writes:
            if b.partial:
                if b.w.get(prod, 0) < cnt:
                    b.w[prod] = cnt
            else:
                b.w = {prod: cnt}
                b.r = {}

    def op(self, E, fns, reads=(), writes=(), ss=False):
        if not isinstance(fns, (list, tuple)):
            fns = [fns]
        self._waits(E, reads, writes, ss)
        ins = None
        for f in fns:
            ins = f()
        ins.then_inc(E.prod.sem, 1)
        E.prod.n += 1
        self._record(E.prod, E.prod.n, reads, writes)

    def dma(self, Q, out_ap, in_ap, reads, writes, sig):
        self._waits(Q, reads, writes)
        ds = self.get_ds(sig, "d_", fresh=(Q is self.pool))
        if Q.seen.get(ds, 0) < ds.floor:
            Q.eng.wait_ge(ds.sem, ds.floor)
            Q.seen[ds] = ds.floor
        Q.eng.dma_start(out=out_ap, in_=in_ap).then_inc(sig.ds.sem, 16)
        sig.ds.n += 16
        self._record(sig.ds, sig.ds.n, reads, writes)

    def coll(self, kind, src, dst):
        Q = self.pool
        self._waits(Q, [src], [dst])
        if dst.ds is None:
            dst.ds = Prod(self.sem("c_" + dst.name), "c_" + dst.name)
            dst.ds.floor = 0
        op = ALU.add if kind == "AllReduce" else ALU.bypass
        Q.eng.collective_compute(kind, op, replica_groups=RG, ins=[src.t.opt()], outs=[dst.t.opt()]).then_inc(dst.ds.sem)
        dst.ds.n += 1
        self._record(dst.ds, dst.ds.n, [src], [dst])

    def rstd_from_ms(self, ms_ps, rs, n=512):
        self.op(self.act, lambda: self.nc.scalar.activation(out=rs[:, 0:n], in_=ms_ps[:, 0:n], func=AF.Sqrt, bias=self.eps_t[:, 0:1], scale=1.0),
                reads=[ms_ps, self.eps_b], writes=[rs])
        self.op(self.dve, lambda: self.nc.vector.reciprocal(out=rs[:, 0:n], in_=rs[:, 0:n]), reads=[rs], writes=[rs])


def build(S, debug=False, stop=99, lite=False):
    k = K(S, debug)
    DEXP = 256 if lite else D_EXP
    nc = k.nc
    TB, NSB = k.TB, k.NSB
    NQ = S // 512
    NKT = S // 128
    PAD = 1024
    with k.st:
        k.mk_engines()
        pe, act, dve, sp, pool = k.pe, k.act, k.dve, k.sp, k.pool

        def ext_in(name, shape, dt=F32):
            return k.dram(name, shape, dt, kind="ExternalInput")

        xT = ext_in("xT", [DM, TB])
        cf32 = ext_in("cf32", [128, 3, 128])
        cbf = ext_in("cbf", [128, 5, 128], BF16)
        cosa = ext_in("cosa", [32, S])
        sina = ext_in("sina", [32, S])
        cosb = ext_in("cosb", [64, S])
        sinb = ext_in("sinb", [64, S])
        L = []
        for i in range(2):
            d = dict(
                an=ext_in(f"an{i}", [128, KC]), fn=ext_in(f"fn{i}", [128, KC]),
                win=ext_in(f"win{i}", [DM, 768]), gains=ext_in(f"gains{i}", [128, 6]),
                lamv=ext_in(f"lamv{i}", [64, 4]), wout=ext_in(f"wout{i}", [256, DM]),
            )
            L.append(d)
        wg0s = ext_in("wg0", [256, D_FF])
        wu0s = ext_in("wu0", [256, D_FF])
        wd0s = ext_in("wd0", [D_FF // 8, DM])
        router = ext_in("router", [128, KC, NEXP])
        weg = ext_in("weg", [DM, DEXP])
        weu = ext_in("weu", [DM, DEXP])
        wed = ext_in("wed", [DEXP, DM])
        outT = k.dram("outT", [DM, TB], F32, kind="ExternalOutput")

        def scratch(name, shape, dt):
            b = k.dram(name, shape, dt)
            if debug:
                k.dbg_out[name] = (b, k.dram("dbg_" + name, shape, dt, kind="ExternalOutput"))
            return b

        h_in = [k.dram(f"h_in{i}", [DM, TB], BF16) for i in range(2)]
        h_g = [k.dram(f"h_g{i}", [NCORES * DM, TB], BF16) for i in range(2)]
        a_in = [k.dram(f"a_in{i}", [256, S], BF16) for i in range(2)]
        a_g = [scratch(f"a_g{i}", [NCORES * 256, S], BF16) for i in range(2)]
        x1d = scratch("x1d", [DM, TB], F32)
        x3d = scratch("x3d", [DM, TB], F32)
        h3_in = k.dram("h3_in", [DM, TB], BF16)
        h3_g = k.dram("h3_g", [NCORES * DM, TB], BF16)
        gt_in = k.dram("gt_in", [NEXP, TB], F32)
        gt_g = scratch("gt_g", [NCORES * NEXP, TB], F32)
        y_in = [k.dram(f"y_in{r}", [DM, TB], F32) for r in range(NCORES)]
        y_r = k.dram("y_r", [NCORES * DM, TB], F32)
        y_rr = [k.dram(f"y_rr{r}", [DM, TB], F32) for r in range(NCORES)]
        y_rb = [k.mkbuf(y_r.t[r * DM:(r + 1) * DM, :], f"y_r{r}", partial=True) for r in range(NCORES)]
        wsh = [None, None]
        win_bf = [k.dram(f"win_bf{i}", [DM, 768], BF16) for i in range(2)]
        wout_f = [k.dram(f"wout_f{i}", [DM, DM], F32) for i in range(2)]
        wout_bf = [k.dram(f"wout_bf{i}", [DM, DM], BF16) for i in range(2)]
        wg0f = k.dram("wg0f", [DM, D_FF], F32)
        wu0f = k.dram("wu0f", [DM, D_FF], F32)
        wd0f = k.dram("wd0f", [D_FF, DM], F32)
        wg0b = k.dram("wg0b", [DM, D_FF], BF16)
        wu0b = k.dram("wu0b", [DM, D_FF], BF16)
        wd0b = k.dram("wd0b", [D_FF, DM], BF16)
        wegb = k.dram("wegb", [DM, DEXP], BF16)
        weub = k.dram("weub", [DM, DEXP], BF16)
        wedb = k.dram("wedb", [DEXP, DM], BF16)

        c32 = k.sb("c32", [128, 3, 128], F32)
        cb = k.sb("cb", [128, 5, 128], BF16)
        onesA = k.sb("onesA", [128, 128], F32)
        onesB = k.sb("onesB", [128, 128], F32)
        onesD = k.sb("onesD", [128, 128], F32)
        ones1 = k.sb("ones1", [128, 128], F32)
        onesb = k.sb("onesb", [128, 128], BF16)
        k.eps_b = k.sb("eps", [128, 1], F32)
        k.eps_t = k.eps_b.t
        nrm = [dict(an=k.sb(f"an_s{i}", [128, KC], F32), fn=k.sb(f"fn_s{i}", [128, KC], F32),
                    gains=k.sb(f"gains_s{i}", [128, 6], F32), lamv=k.sb(f"lamv_s{i}", [64, 4], F32),
                    nlam=k.sb(f"nlam{i}", [128, 1], F32), dog=k.sb(f"dog{i}", [128, 1], F32)) for i in range(2)]
        rt_s = k.sb("rt_s", [128, KC, NEXP], F32)

        k.dma(sp, c32[:], cf32.t, [cf32], [c32], c32)
        k.dma(sp, cb[:], cbf.t, [cbf], [cb], cb)
        k.dma(sp, rt_s[:], router.t, [router], [rt_s], rt_s)
        for i in range(2):
            for nm in ("an", "fn", "gains", "lamv"):
                k.dma(sp, nrm[i][nm][:], L[i][nm].t, [L[i][nm]], [nrm[i][nm]], nrm[i][nm])
        k.op(dve, lambda: nc.vector.memset(onesA[:], 0.0), writes=[onesA])
        k.op(dve, [lambda: nc.vector.memset(onesA[0:64, 0:64], 1.0 / 64), lambda: nc.vector.memset(onesA[64:128, 64:128], 1.0 / 64)], writes=[onesA], ss=True)
        k.op(dve, lambda: nc.vector.memset(onesB[:], 1.0 / 128), writes=[onesB])
        k.op(dve, lambda: nc.vector.memset(onesD[:], 1.0 / DM), writes=[onesD])
        k.op(dve, lambda: nc.vector.memset(ones1[:], 1.0), writes=[ones1])
        k.op(dve, lambda: nc.vector.memset(onesb[:], 1.0), writes=[onesb])
        k.op(dve, lambda: nc.vector.memset(k.eps_t[:], EPS), writes=[k.eps_b])
        rotA, rotB, ident32 = 0, 1, 2
        M_LO, M_HI, M_LO0, M_HIL, IDB = 0, 1, 2, 3, 4

        def cast_rows(src, dst, rows, blk=256):
            for r0 in range(0, rows, blk):
                r1 = min(rows, r0 + blk)
                k.dma(pool, dst.t[r0:r1, :], src.t[r0:r1, :], [src], [dst], dst)
        for i in range(2):
            cast_rows(L[i]["win"], win_bf[i], DM)
        def gather_cast(src_ext, shard_shape, full_f, full_b, nm):
            bounce = k.dram("bnc_" + nm, shard_shape, F32)
            k.dma(pool, bounce.t, src_ext.t, [src_ext], [bounce], bounce)
            k.coll("AllGather", bounce, full_f)
            cast_rows(full_f, full_b, shard_shape[0] * NCORES)
        gather_cast(L[0]["wout"], [256, DM], wout_f[0], wout_bf[0], "wo0")

        def rmsnorm_tile(xt, g_s, hb, stack_ps, pools, hf_cb=None):
            ms = pools["ms_ps"]
            for kc in range(KC):
                sq = pools["sq"][kc % 2]
                k.op(act, lambda kc=kc, sq=sq: nc.scalar.activation(out=sq[:], in_=xt[:, kc, :], func=AF.Square), reads=[xt], writes=[sq])
                k.op(pe, lambda kc=kc, sq=sq: nc.tensor.matmul(ms[:], lhsT=onesD[:], rhs=sq[:], start=(kc == 0), stop=(kc == KC - 1)),
                     reads=[sq, onesD], writes=[ms])
            rs = pools["rs"]
            k.rstd_from_ms(ms, rs)
            for kc in range(KC):
                if hf_cb is None:
                    k.op(dve, lambda kc=kc: nc.vector.scalar_tensor_tensor(out=hb[:, kc, :], in0=xt[:, kc, :], scalar=g_s[:, kc:kc + 1], in1=rs[:],
                                                                          op0=ALU.mult, op1=ALU.mult), reads=[xt, g_s, rs], writes=[hb])
                else:
                    hf = pools["sq"][kc % 2]
                    k.op(dve, lambda kc=kc, hf=hf: nc.vector.scalar_tensor_tensor(out=hf[:], in0=xt[:, kc, :], scalar=g_s[:, kc:kc + 1], in1=rs[:],
                                                                                op0=ALU.mult, op1=ALU.mult), reads=[xt, g_s, rs], writes=[hf])
                    k.op(act, lambda kc=kc, hf=hf: nc.scalar.copy(out=hb[:, kc, :], in_=hf[:]), reads=[hf], writes=[hb])
                    hf_cb(kc, hf)

        def phase_norm0():
            with k.scope() as s:
                xt = [k.sb(f"n0x{j}", [128, KC, 512], F32, s) for j in range(2)]
                hb = [k.sb(f"n0h{j}", [128, KC, 512], BF16, s) for j in range(2)]
                pools = dict(sq=[k.sb(f"n0sq{j}", [128, 512], F32, s) for j in range(2)], rs=k.sb("n0rs", [128, 512], F32, s),
                             ms_ps=k.ps("n0ms", s))
                for sbk in range(NSB):
                    x_, h_ = xt[sbk % 2], hb[sbk % 2]
                    k.dma(sp, x_[:], xT.t[:, sbk * 512:(sbk + 1) * 512].rearrange("(c p) t -> p c t", p=128), [xT], [x_], x_)
                    rmsnorm_tile(x_, nrm[0]["an"], h_, s, pools)
                    k.dma(sp, h_in[0].t[:, sbk * 512:(sbk + 1) * 512].rearrange("(c p) t -> p c t", p=128), h_[:], [h_], [h_in[0]], h_)
            k.coll("AllGather", h_in[0], h_g[0])

        def lam_setup(i):
            lam_init = 0.8 - 0.6 * math.exp(-0.3 * i)
            with k.scope() as s:
                pr = k.sb("lampr", [64, 2], F32, s)
                ee = k.sb("lamee", [128, 2], F32, s)
                lp = k.ps("lamps", s, (128, 2))
                lv = nrm[i]["lamv"]
                k.op(dve, [lambda: nc.vector.tensor_tensor(out=pr[:, 0:1], in0=lv[:, 0:1], in1=lv[:, 1:2], op=ALU.mult),
                           lambda: nc.vector.tensor_tensor(out=pr[:, 1:2], in0=lv[:, 2:3], in1=lv[:, 3:4], op=ALU.mult)], reads=[lv], writes=[pr])
                k.op(pe, lambda: nc.tensor.matmul(lp[:], lhsT=ones1[0:64, :], rhs=pr[:], start=True, stop=True), reads=[pr, ones1], writes=[lp])
                k.op(act, lambda: nc.scalar.activation(out=ee[:], in_=lp[:], func=AF.Exp), reads=[lp], writes=[ee])
                nl = nrm[i]["nlam"]
                k.op(dve, lambda: nc.vector.tensor_tensor(out=nl[:], in0=ee[:, 1:2], in1=ee[:, 0:1], op=ALU.subtract), reads=[ee], writes=[nl])
                k.op(dve, lambda: nc.vector.tensor_scalar(out=nl[:], in0=nl[:], scalar1=-lam_init, scalar2=None, op0=ALU.add), reads=[nl], writes=[nl], ss=True)
                k.op(dve, lambda: nc.vector.tensor_scalar(out=nrm[i]["dog"][:], in0=nrm[i]["gains"][:, 4:5], scalar1=(1.0 - lam_init), scalar2=None, op0=ALU.mult),
                     reads=[nrm[i]["gains"]], writes=[nrm[i]["dog"]])

        def project(i, s, col0, ncol_chunks, dests, wsb, hpool, post):
            pp = [k.ps(f"prj{j}", s) for j in range(2)]
            cnt = 0
            for qi in range(NQ):
                r, t0 = divmod(qi * 512, TB)
                hb = hpool[qi % 2]
                k.dma(sp, hb[:], h_g[i].t[r * DM:(r + 1) * DM, t0:t0 + 512].rearrange("(c p) t -> p c t", p=128), [h_g[i]], [hb], hb)
                for j in range(ncol_chunks):
                    pb = pp[cnt % 2]
                    cnt += 1
                    c0 = col0 + j * 128
                    k.op(pe, [(lambda kc=kc, pb=pb, c0=c0: nc.tensor.matmul(pb[:], lhsT=wsb[:, kc, c0:c0 + 128], rhs=hb[:, kc, :], start=(kc == 0), stop=(kc == KC - 1)))
                              for kc in range(KC)], reads=[wsb, hb], writes=[pb])
                    post(j, qi, pb)

        def load_w_in(i, s):
            wsb = k.sb("winsb", [128, KC, 768], BF16, s)
            k.dma(sp, wsb[:], win_bf[i].t.rearrange("(c p) n -> p c n", p=128), [win_bf[i]], [wsb], wsb)
            return wsb

        def out_norm_store(s, o32, gain_ap, gain_buf, row0, i, qcols, pools):
            sq, ax, rs, ob = pools["sq"], pools["ax"], pools["rs"], pools["ob"]
            k.op(act, lambda: nc.scalar.activation(out=sq[:], in_=o32[:], func=AF.Square), reads=[o32], writes=[sq])
            k.op(pe, lambda: nc.tensor.matmul(ax[:], lhsT=onesB[:], rhs=sq[:], start=True, stop=True), reads=[onesB, sq], writes=[ax])
            k.rstd_from_ms(ax, rs)
            k.op(dve, lambda: nc.vector.scalar_tensor_tensor(out=ob[:], in0=o32[:], scalar=gain_ap, in1=rs[:], op0=ALU.mult, op1=ALU.mult),
                 reads=[o32, gain_buf, rs], writes=[ob])
            k.dma(sp, a_in[i].t[row0:row0 + 128, qcols], ob[:], [ob], [a_in[i]], ob)

        def load_cs(qi, cpool, is_a):
            cs = cpool[qi % 2]
            c_, s_ = cs
            sl = slice(qi * 512, (qi + 1) * 512)
            if is_a:
                for g in range(4):
                    k.dma(sp, c_[g * 32:(g + 1) * 32, :], cosa.t[:, sl], [cosa], [c_], c_)
                    k.dma(sp, s_[g * 32:(g + 1) * 32, :], sina.t[:, sl], [sina], [s_], s_)
            else:
                for g in range(2):
                    k.dma(sp, c_[g * 64:(g + 1) * 64, :], cosb.t[:, sl], [cosb], [c_], c_)
                    k.dma(sp, s_[g * 64:(g + 1) * 64, :], sinb.t[:, sl], [sinb], [s_], s_)
            return cs

        def phase_diff(i):
            with k.scope() as s:
                qaT = k.sb("qaT", [128, S], BF16, s, partial=True)
                kaT = k.sb("kaT", [128, S], BF16, s, partial=True)
                vtok = k.sb("vtok", [128, NKT, 128], BF16, s, partial=True)
                with k.scope() as s2:
                    wsb = load_w_in(i, s2)
                    hpool = [k.sb(f"dh{j}", [128, KC, 512], BF16, s2) for j in range(2)]
                    cpool = [(k.sb(f"dc{j}", [128, 512], F32, s2, partial=True), k.sb(f"dsn{j}", [128, 512], F32, s2, partial=True)) for j in range(2)]
                    vch = [k.sb(f"dvch{j}", [128, 512], BF16, s2) for j in range(2)]
                    tp = k.ps_split("vtp", s2, 2, 128, BF16)
                    ws_d = mk_qk_ws(s2, "da")
                    cur = {}

                    def post(j, qi, pb):
                        if j == 0:
                            cur["cs"] = load_cs(qi, cpool, True)
                        if j < 2:
                            _qk(j, qi, pb, (qaT, kaT)[j], 0, onesA, rotA, cur["cs"], i, ws_d)
                        else:
                            vc = vch[qi % 2]
                            k.op(act, lambda: nc.scalar.copy(out=vc[:], in_=pb[:]), reads=[pb], writes=[vc])
                            for t in range(4):
                                tb = tp[t % 2]
                                k.op(pe, lambda t=t, tb=tb: nc.tensor.transpose(tb[:], vc[:, t * 128:(t + 1) * 128], cb[:, IDB, :]), reads=[vc, cb], writes=[tb])
                                k.op(dve, lambda t=t, tb=tb: nc.vector.tensor_copy(out=vtok[:, qi * 4 + t, :], in_=tb[:]), reads=[tb], writes=[vtok])
                    project(i, s2, 0, 3, None, wsb, hpool, post)
                with k.scope() as s3:
                    sps = [[k.ps(f"ss{m}{j}", s3) for j in range(2)] for m in range(2)]
                    ops_ = [k.ps(f"oo{m}", s3) for m in range(2)]
                    dps = [k.ps(f"dd{m}", s3) for m in range(2)]
                    pts = [[k.sb(f"pt{m}{j}", [128, 512], BF16, s3) for j in range(3)] for m in range(2)]
                    r_ = [k.sb(f"dr{m}", [128, 512], F32, s3) for m in range(2)]
                    ab = [k.sb(f"dab{m}", [128, 512], F32, s3) for m in range(2)]
                    o32 = k.sb("do32", [128, 512], F32, s3)
                    pools = dict(sq=k.sb("dosq", [128, 512], F32, s3), rs=k.sb("dors", [128, 512], F32, s3), ob=k.sb("doob", [128, 512], BF16, s3))
                    step = 0
                    for qi in range(NQ):
                        qs = slice(qi * 512, (qi + 1) * 512)
                        for kt in range(NKT):
                            ks = slice(kt * 128, (kt + 1) * 128)
                            for m in range(2):
                                sp_ = sps[m][step % 2]
                                pt = pts[m][step % 3]
                                rows = slice(m * 64, (m + 1) * 64)
                                k.op(pe, lambda sp_=sp_, rows=rows, ks=ks: nc.tensor.matmul(sp_[:], lhsT=kaT[rows, ks], rhs=qaT[rows, qs], start=True, stop=True),
                                     reads=[kaT, qaT], writes=[sp_])
                                k.op(act, lambda sp_=sp_, pt=pt: nc.scalar.activation(out=pt[:], in_=sp_[:], func=AF.Exp, scale=0.125), reads=[sp_], writes=[pt])
                                k.op(pe, [lambda pt=pt, m=m, kt=kt: nc.tensor.matmul(ops_[m][:], lhsT=vtok[:, kt, :], rhs=pt[:], start=(kt == 0), stop=(kt == NKT - 1)),
                                          lambda pt=pt, m=m, kt=kt: nc.tensor.matmul(dps[m][:], lhsT=onesb[:], rhs=pt[:], start=(kt == 0), stop=(kt == NKT - 1))],
                                     reads=[vtok, pt, onesb], writes=[ops_[m], dps[m]])
                            step += 1
                        for m in range(2):
                            k.op(dve, lambda m=m: nc.vector.reciprocal(out=r_[m][:], in_=dps[m][:]), reads=[dps[m]], writes=[r_[m]])
                            k.op(dve, lambda m=m: nc.vector.tensor_tensor(out=ab[m][:], in0=ops_[m][:], in1=r_[m][:], op=ALU.mult), reads=[ops_[m], r_[m]], writes=[ab[m]])
                        nl = nrm[i]["nlam"]
                        k.op(dve, lambda: nc.vector.scalar_tensor_tensor(out=o32[:], in0=ab[1][:], scalar=nl[:, 0:1], in1=ab[0][:], op0=ALU.mult, op1=ALU.add),
                             reads=[ab[0], ab[1], nl], writes=[o32])
                        pools["ax"] = sps[0][step % 2]
                        out_norm_store(s3, o32, nrm[i]["dog"][:, 0:1], nrm[i]["dog"], 0, i, qs, pools)

        def mk_qk_ws(s, nm):
            return dict(sq=[k.sb(f"{nm}sq{j}", [128, 512], F32, s) for j in range(2)], qn=[k.sb(f"{nm}qn{j}", [128, 512], F32, s) for j in range(2)],
                        rs=[k.sb(f"{nm}rs{j}", [128, 512], F32, s) for j in range(2)], t1=[k.sb(f"{nm}t1{j}", [128, 512], F32, s) for j in range(2)],
                        aux=[k.ps(f"{nm}aux{j}", s) for j in range(2)], n=0)

        def _qk(gidx, qi, pb, dst, dst_off, ones_m, rot_idx, cs, i_layer, ws):
            n = ws["n"]
            ws["n"] += 1
            sq_, qn_, rs_, t1_, ax = ws["sq"][n % 2], ws["qn"][n % 2], ws["rs"][n % 2], ws["t1"][n % 2], ws["aux"][n % 2]
            g_s = nrm[i_layer]["gains"]
            k.op(act, lambda: nc.scalar.activation(out=sq_[:], in_=pb[:], func=AF.Square), reads=[pb], writes=[sq_])
            k.op(pe, lambda: nc.tensor.matmul(ax[:], lhsT=ones_m[:], rhs=sq_[:], start=True, stop=True), reads=[ones_m, sq_], writes=[ax])
            k.rstd_from_ms(ax, rs_)
            k.op(dve, lambda: nc.vector.scalar_tensor_tensor(out=qn_[:], in0=pb[:], scalar=g_s[:, gidx:gidx + 1], in1=rs_[:], op0=ALU.mult, op1=ALU.mult),
                 reads=[pb, g_s, rs_], writes=[qn_])
            k.op(pe, lambda: nc.tensor.matmul(ax[:], lhsT=c32[:, rot_idx, :], rhs=qn_[:], start=True, stop=True), reads=[c32, qn_], writes=[ax])
            cosb_, sinb_ = cs
            k.op(dve, lambda: nc.vector.tensor_tensor(out=t1_[:], in0=qn_[:], in1=cosb_[:], op=ALU.mult), reads=[qn_, cosb_], writes=[t1_])
            k.op(dve, lambda: nc.vector.tensor_tensor(out=qn_[:], in0=ax[:], in1=sinb_[:], op=ALU.mult), reads=[ax, sinb_], writes=[qn_])
            k.op(dve, lambda: nc.vector.tensor_tensor(out=dst[:, dst_off + qi * 512:dst_off + (qi + 1) * 512], in0=t1_[:], in1=qn_[:], op=ALU.add),
                 reads=[t1_, qn_], writes=[dst])

        def phase_dil(i):
            SP_ = S + 2 * PAD
            QR = min(S, 4096)
            with k.scope() as s:
                qbT = k.sb("qbT", [128, S], BF16, s, partial=True)
                kbT = k.sb("kbT", [128, SP_], BF16, s, partial=True)
                vbT = k.sb("vbT", [128, SP_], BF16, s, partial=True)
                k.op(dve, [lambda: nc.vector.memset(kbT[:, 0:PAD], 0.0), lambda: nc.vector.memset(kbT[:, PAD + S:SP_], 0.0),
                           lambda: nc.vector.memset(vbT[:, 0:PAD], 0.0), lambda: nc.vector.memset(vbT[:, PAD + S:SP_], 0.0)], writes=[kbT, vbT])
                with k.scope() as s2:
                    wsb = load_w_in(i, s2)
                    hpool = [k.sb(f"bh{j}", [128, KC, 512], BF16, s2) for j in range(2)]
                    cpool = [(k.sb(f"bc{j}", [128, 512], F32, s2, partial=True), k.sb(f"bsn{j}", [128, 512], F32, s2, partial=True)) for j in range(2)]
                    ws_b = mk_qk_ws(s2, "db")
                    cur = {}

                    def post(j, qi, pb):
                        if j == 0:
                            cur["cs"] = load_cs(qi, cpool, False)
                        if j == 0:
                            _qk(2, qi, pb, qbT, 0, onesB, rotB, cur["cs"], i, ws_b)
                        elif j == 1:
                            _qk(3, qi, pb, kbT, PAD, onesB, rotB, cur["cs"], i, ws_b)
                        else:
                            k.op(act, lambda: nc.scalar.copy(out=vbT[:, PAD + qi * 512:PAD + (qi + 1) * 512], in_=pb[:]), reads=[pb], writes=[vbT])
                    project(i, s2, 384, 3, None, wsb, hpool, post)
                with k.scope() as s3:
                    oacc = k.sb("oacc", [128, QR], F32, s3, partial=True)
                    dacc = k.sb("dacc", [128, QR], F32, s3, partial=True)
                    sps = k.ps_split("bs", s3, 2, 128, F32)
                    ops_ = k.ps_split("bo", s3, 2, 128, F32)
                    dps = k.ps_split("bd", s3, 2, 128, F32)
                    tps = k.ps_split("bt", s3, 1, 128, BF16)
                    pf = [k.sb(f"bpf{j}", [128, 128], BF16, s3) for j in range(3)]
                    pm = [k.sb(f"bpm{j}", [128, 128], BF16, s3) for j in range(3)]
                    vt = [k.sb(f"bvt{j}", [128, 128], BF16, s3) for j in range(3)]
                    o32 = k.sb("bo32", [128, 512], F32, s3)
                    rd = k.sb("brd", [128, 512], F32, s3)
                    pools = dict(sq=k.sb("bosq", [128, 512], F32, s3), rs=k.sb("bors", [128, 512], F32, s3), ob=k.sb("boob", [128, 512], BF16, s3),
                                 ax=k.ps("boax", s3))
                    scale = 128 ** -0.5
                    cnt = dict(q=0, v=0, p=0)
                    for t0 in range(0, S, QR):
                        first = True
                        for dil in (1, 4, 16):
                            Ld = S // dil
                            nqt = QR // dil // 128
                            for r in range(dil):
                                mbase = t0 // dil
                                vtiles = {}

                                def get_v(j, dil=dil, r=r, mbase=mbase, vtiles=vtiles):
                                    if j in vtiles:
                                        return vtiles[j]
                                    n0 = mbase + 128 * j - 64
                                    c0 = PAD + r + dil * n0
                                    tb = tps[0]
                                    vb = vt[cnt["v"] % 3]
                                    cnt["v"] += 1
                                    k.op(pe, lambda: nc.tensor.transpose(tb[:], vbT[:, c0:c0 + 127 * dil + 1:dil], cb[:, IDB, :]), reads=[vbT, cb], writes=[tb])
                                    k.op(dve, lambda: nc.vector.tensor_copy(out=vb[:], in_=tb[:]), reads=[tb], writes=[vb])
                                    vtiles[j] = (vb, c0)
                                    return vtiles[j]
                                for qt in range(nqt):
                                    m0 = mbase + 128 * qt
                                    qc0 = r + dil * m0
                                    ob_, db_ = ops_[cnt["q"] % 2], dps[cnt["q"] % 2]
                                    cnt["q"] += 1
                                    for half in range(2):
                                        vb, c0 = get_v(qt + half)
                                        if half == 0:
                                            mk = M_LO0 if m0 == 0 else M_LO
                                        else:
                                            mk = M_HIL if m0 == Ld - 128 else M_HI
                                        sp_ = sps[cnt["p"] % 2]
                                        pf_ = pf[cnt["p"] % 3]
                                        pm_ = pm[cnt["p"] % 3]
                                        cnt["p"] += 1
                                        k.op(pe, lambda sp_=sp_, c0=c0: nc.tensor.matmul(sp_[:], lhsT=kbT[:, c0:c0 + 127 * dil + 1:dil], rhs=qbT[:, qc0:qc0 + 127 * dil + 1:dil],
                                                                                          start=True, stop=True), reads=[kbT, qbT], writes=[sp_])
                                        k.op(act, lambda sp_=sp_, pf_=pf_: nc.scalar.activation(out=pf_[:], in_=sp_[:], func=AF.Exp, scale=scale), reads=[sp_], writes=[pf_])
                                        k.op(dve, lambda pf_=pf_, pm_=pm_, mk=mk: nc.vector.tensor_tensor(out=pm_[:], in0=pf_[:], in1=cb[:, mk, :], op=ALU.mult),
                                             reads=[pf_, cb], writes=[pm_])
                                        k.op(pe, [lambda vb=vb, pm_=pm_, half=half: nc.tensor.matmul(ob_[:], lhsT=vb[:], rhs=pm_[:], start=(half == 0), stop=(half == 1)),
                                                  lambda pm_=pm_, half=half: nc.tensor.matmul(db_[:], lhsT=onesb[:], rhs=pm_[:], start=(half == 0), stop=(half == 1))],
                                             reads=[vb, pm_, onesb], writes=[ob_, db_])
                                    lc0 = qc0 - t0
                                    osl = oacc[:, lc0:lc0 + 127 * dil + 1:dil]
                                    dsl = dacc[:, lc0:lc0 + 127 * dil + 1:dil]
                                    if first:
                                        k.op(dve, [lambda: nc.vector.tensor_copy(out=osl, in_=ob_[:]), lambda: nc.vector.tensor_copy(out=dsl, in_=db_[:])],
                                             reads=[ob_, db_], writes=[oacc, dacc])
                                    else:
                                        k.op(dve, [lambda: nc.vector.tensor_tensor(out=osl, in0=ob_[:], in1=osl, op=ALU.add),
                                                   lambda: nc.vector.tensor_tensor(out=dsl, in0=db_[:], in1=dsl, op=ALU.add)],
                                             reads=[ob_, db_, oacc, dacc], writes=[oacc, dacc])
                            first = False
                        for c in range(QR // 512):
                            cs_ = slice(c * 512, (c + 1) * 512)
                            k.op(dve, lambda: nc.vector.reciprocal(out=rd[:], in_=dacc[:, cs_]), reads=[dacc], writes=[rd])
                            k.op(dve, lambda: nc.vector.tensor_tensor(out=o32[:], in0=oacc[:, cs_], in1=rd[:], op=ALU.mult), reads=[oacc, rd], writes=[o32])
                            out_norm_store(s3, o32, nrm[i]["gains"][:, 5:6], nrm[i]["gains"], 128, i, slice(t0 + c * 512, t0 + (c + 1) * 512), pools)

        def stream_mm(s, wdram, KCH, ncols, wpool, wcnt, rhs_buf, rhs_reads, consume, pps, pcnt, group=256):
            for g0 in range(0, ncols, group):
                wb = wpool[wcnt[0] % len(wpool)]
                wcnt[0] += 1
                gw = min(group, ncols - g0)
                k.dma(sp, wb[:, 0:KCH, 0:gw], wdram.t[:, g0:g0 + gw].rearrange("(c p) n -> p c n", p=128), [wdram], [wb], wb)
                for j in range(gw // 128):
                    pb = pps[pcnt[0] % len(pps)]
                    pcnt[0] += 1
                    k.op(pe, [(lambda kc=kc, j=j, pb=pb, wb=wb: nc.tensor.matmul(pb[:], lhsT=wb[:, kc, j * 128:(j + 1) * 128], rhs=rhs_buf[:, kc, :],
                                                                                 start=(kc == 0), stop=(kc == KCH - 1))) for kc in range(KCH)],
                         reads=[wb] + rhs_reads, writes=[pb])
                    consume((g0 // 128) + j, pb)

        def phase_tok(i):
            k.coll("AllGather", a_in[i], a_g[i])
            x_src = xT if i == 0 else x1d
            with k.scope() as s:
                xt = [k.sb(f"tx{j}", [128, KC, 512], F32, s) for j in range(1)]
                asb = [k.sb(f"ta{j}", [128, KC, 512], BF16, s) for j in range(1)]
                h2 = k.sb("th2", [128, KC, 512], BF16, s)
                wpool = [k.sb(f"tw{j}", [128, 44, 256], BF16, s) for j in range(2)]
                pps = [k.ps(f"tp{j}", s) for j in range(4 if i == 0 else 2)]
                NPP = len(pps)
                npool = dict(sq=[k.sb(f"tsq{j}", [128, 512], F32, s) for j in range(2)], rs=k.sb("trs", [128, 512], F32, s), ms_ps=k.ps("tms", s))
                wcnt, pcnt = [0], [0]
                if i == 0:
                    actb = k.sb("tact", [128, D_FF // 128, 512], BF16, s)
                    sg = [k.sb(f"tsg{j}", [128, 512], F32, s) for j in range(2)]
                    hn = h2
                else:
                    lg_ps = [k.ps(f"tlg{tt}", s, (128, NEXP)) for tt in range(4)]
                    gT_ps = k.ps("tgt", s, (NEXP, 512))
                    lgs = k.sb("tlgs", [128, 4, NEXP], F32, s)
                    mx8 = k.sb("tmx8", [128, 4, 8], F32, s)
                    msk = k.sb("tmsk", [128, 4, NEXP], F32, s)
                    ee = k.sb("tee", [128, 4, NEXP], F32, s)
                    nv1 = k.sb("tnv1", [128, 4], F32, s)
                    den = k.sb("tden", [128, 4], F32, s)
                    gts = k.sb("tgts", [NEXP, 512], F32, s)
                for sbk in range(NSB):
                    x_, a_ = xt[0], asb[0]
                    tsl = slice(sbk * 512, (sbk + 1) * 512)
                    k.dma(sp, x_[:], x_src.t[:, tsl].rearrange("(c p) t -> p c t", p=128), [x_src], [x_], x_)
                    k.dma(sp, a_[:], a_g[i].t.rearrange("(c p) t -> p c t", p=128)[:, :, bass.ds(k.pid_sp * TB + sbk * 512, 512)], [a_g[i]], [a_], a_)
                    amap = [2 * r for r in range(8)] + [2 * r + 1 for r in range(8)]
                    for g0 in range(0, DM, 256):
                        wb = wpool[wcnt[0] % 2]
                        wcnt[0] += 1
                        k.dma(sp, wb[:, 0:KC, :], wout_bf[i].t[:, g0:g0 + 256].rearrange("(c p) n -> p c n", p=128), [wout_bf[i]], [wb], wb)
                        for j in range(2):
                            pb = pps[pcnt[0] % NPP]
                            pcnt[0] += 1
                            oc = g0 // 128 + j
                            k.op(pe, [(lambda kc=kc, j=j, pb=pb, wb=wb: nc.tensor.matmul(pb[:], lhsT=wb[:, kc, j * 128:(j + 1) * 128], rhs=a_[:, amap[kc], :],
                                                                                         start=(kc == 0), stop=(kc == KC - 1))) for kc in range(KC)],
                                 reads=[wb, a_], writes=[pb])
                            k.op(dve, lambda oc=oc, pb=pb: nc.vector.tensor_tensor(out=x_[:, oc, :], in0=pb[:], in1=x_[:, oc, :], op=ALU.add), reads=[pb, x_], writes=[x_])
                    if i == 0:
                        rmsnorm_tile(x_, nrm[0]["fn"], h2, s, npool)
                        for g0 in range(0, D_FF, 256):
                            wb = wpool[wcnt[0] % 2]
                            wcnt[0] += 1
                            k.dma(sp, wb[:, 0:KC, :], wg0b.t[:, g0:g0 + 256].rearrange("(c p) n -> p c n", p=128), [wg0b], [wb], wb)
                            k.dma(sp, wb[:, KC:2 * KC, :], wu0b.t[:, g0:g0 + 256].rearrange("(c p) n -> p c n", p=128), [wu0b], [wb], wb)
                            for j in range(2):
                                fc = g0 // 128 + j
                                pg = pps[pcnt[0] % NPP]
                                pu = pps[(pcnt[0] + 1) % NPP]
                                pcnt[0] += 2
                                k.op(pe, [(lambda kc=kc, j=j, pg=pg, wb=wb: nc.tensor.matmul(pg[:], lhsT=wb[:, kc, j * 128:(j + 1) * 128], rhs=h2[:, kc, :],
                                                                                             start=(kc == 0), stop=(kc == KC - 1))) for kc in range(KC)],
                                     reads=[wb, h2], writes=[pg])
                                k.op(pe, [(lambda kc=kc, j=j, pu=pu, wb=wb: nc.tensor.matmul(pu[:], lhsT=wb[:, KC + kc, j * 128:(j + 1) * 128], rhs=h2[:, kc, :],
                                                                                             start=(kc == 0), stop=(kc == KC - 1))) for kc in range(KC)],
                                     reads=[wb, h2], writes=[pu])
                                sg_ = sg[fc % 2]
                                k.op(act, lambda pg=pg, sg_=sg_: nc.scalar.activation(out=sg_[:], in_=pg[:], func=AF.Silu), reads=[pg], writes=[sg_])
                                k.op(dve, lambda fc=fc, pu=pu, sg_=sg_: nc.vector.tensor_tensor(out=actb[:, fc, :], in0=pu[:], in1=sg_[:], op=ALU.mult),
                                     reads=[pu, sg_], writes=[actb])
                        for g0 in range(0, DM, 256):
                            wb = wpool[wcnt[0] % 2]
                            wcnt[0] += 1
                            for c0 in range(0, 44, 11):
                                k.dma(sp, wb[:, c0:c0 + 11, :], wd0b.t[c0 * 128:(c0 + 11) * 128, g0:g0 + 256].rearrange("(c p) n -> p c n", p=128), [wd0b], [wb], wb)
                            for j in range(2):
                                pb = pps[pcnt[0] % NPP]
                                pcnt[0] += 1
                                oc = g0 // 128 + j
                                k.op(pe, [(lambda fc=fc, j=j, pb=pb, wb=wb: nc.tensor.matmul(pb[:], lhsT=wb[:, fc, j * 128:(j + 1) * 128], rhs=actb[:, fc, :],
                                                                                             start=(fc == 0), stop=(fc == 43))) for fc in range(44)],
                                     reads=[wb, actb], writes=[pb])
                                k.op(dve, lambda oc=oc, pb=pb: nc.vector.tensor_tensor(out=x_[:, oc, :], in0=pb[:], in1=x_[:, oc, :], op=ALU.add), reads=[pb, x_], writes=[x_])
                        k.dma(sp, x1d.t[:, tsl].rearrange("(c p) t -> p c t", p=128), x_[:], [x_], [x1d], x_)
                        rmsnorm_tile(x_, nrm[1]["an"], hn, s, npool)
                        k.dma(sp, h_in[1].t[:, tsl].rearrange("(c p) t -> p c t", p=128), hn[:], [hn], [h_in[1]], hn)
                    else:
                        k.dma(sp, x3d.t[:, tsl].rearrange("(c p) t -> p c t", p=128), x_[:], [x_], [x3d], x_)

                        def hf_cb(kc, hf):
                            k.op(pe, [(lambda tt=tt: nc.tensor.matmul(lg_ps[tt][:], lhsT=hf[:, tt * 128:(tt + 1) * 128], rhs=rt_s[:, kc, :],
                                                                      start=(kc == 0), stop=(kc == KC - 1))) for tt in range(4)], reads=[hf, rt_s], writes=lg_ps)
                        rmsnorm_tile(x_, nrm[1]["fn"], h2, s, npool, hf_cb=hf_cb)
                        k.dma(sp, h3_in.t[:, tsl].rearrange("(c p) t -> p c t", p=128), h2[:], [h2], [h3_in], h2)
                        k.op(dve, [(lambda tt=tt: nc.vector.tensor_copy(out=lgs[:, tt, :], in_=lg_ps[tt][:])) for tt in range(4)], reads=lg_ps, writes=[lgs], ss=True)
                        k.op(dve, [(lambda tt=tt: nc.vector.max(out=mx8[:, tt, :], in_=lgs[:, tt, :])) for tt in range(4)], reads=[lgs], writes=[mx8], ss=True)
                        k.op(dve, [(lambda tt=tt: nc.vector.tensor_scalar(out=msk[:, tt, :], in0=lgs[:, tt, :], scalar1=mx8[:, tt, 1:2], scalar2=None, op0=ALU.is_ge)) for tt in range(4)]
                             + [lambda: nc.vector.tensor_scalar(out=nv1[:], in0=mx8[:, :, 0], scalar1=-1.0, scalar2=None, op0=ALU.mult)], reads=[lgs, mx8], writes=[msk, nv1], ss=True)
                        k.op(act, [(lambda tt=tt: nc.scalar.activation(out=ee[:, tt, :], in_=lgs[:, tt, :], func=AF.Exp, bias=nv1[:, tt:tt + 1], scale=1.0)) for tt in range(4)],
                             reads=[lgs, nv1], writes=[ee], ss=True)
                        k.op(dve, lambda: nc.vector.tensor_tensor(out=ee[:], in0=ee[:], in1=msk[:], op=ALU.mult), reads=[ee, msk], writes=[ee], ss=True)
                        k.op(dve, lambda: nc.vector.tensor_reduce(out=den[:], in_=ee[:], axis=AX.X, op=ALU.add), reads=[ee], writes=[den], ss=True)
                        k.op(dve, lambda: nc.vector.reciprocal(out=den[:], in_=den[:]), reads=[den], writes=[den], ss=True)
                        k.op(dve, [(lambda tt=tt: nc.vector.tensor_scalar(out=ee[:, tt, :], in0=ee[:, tt, :], scalar1=den[:, tt:tt + 1], scalar2=None, op0=ALU.mult)) for tt in range(4)],
                             reads=[ee, den], writes=[ee], ss=True)
                        k.op(pe, [(lambda tt=tt: nc.tensor.matmul(gT_ps[:, tt * 128:(tt + 1) * 128], lhsT=ee[:, tt, :], rhs=c32[:, ident32, :], start=True, stop=True)) for tt in range(4)],
                             reads=[ee, c32], writes=[gT_ps])
                        k.op(dve, lambda: nc.vector.tensor_copy(out=gts[:], in_=gT_ps[:]), reads=[gT_ps], writes=[gts], ss=True)
                        k.dma(sp, gt_in.t[:, tsl], gts[:], [gts], [gt_in], gts)
            if i == 0:
                k.coll("AllGather", h_in[1], h_g[1])

        def phase_moe():
            k.coll("AllGather", h3_in, h3_g)
            k.coll("AllGather", gt_in, gt_g)
            NFC = DEXP // 128
            with k.scope() as s:
                hb = [k.sb(f"mh{j}", [128, KC, 512], BF16, s) for j in range(2)]
                actb = k.sb("mact", [128, NFC, 512], BF16, s)
                wpool = [k.sb(f"mw{j}", [128, max(NFC, 2 * KC), 256], BF16, s) for j in range(2)]
                pps = [k.ps(f"mp{j}", s) for j in range(6)]
                gps = k.ps("mgp", s)
                grow = [k.sb(f"mgr{j}", [1, TB], F32, s) for j in range(2)]
                gbc = [k.sb(f"mgb{j}", [128, 512], F32, s) for j in range(2)]
                sg = [k.sb(f"msg{j}", [128, 512], F32, s) for j in range(3)]
                ysb = [k.sb(f"my{j}", [128, 512], F32, s) for j in range(3)]
                wcnt, pcnt, ycnt = [0], [0], [0]
                gview = gt_g.t.rearrange("(r e) t -> e r t", e=NEXP)
                for tg in range(NQ):
                    r, t0 = divmod(tg * 512, TB)
                    h_ = hb[tg % 2]
                    gr, gb = grow[r % 2], gbc[tg % 2]
                    k.dma(sp, h_[:], h3_g.t[r * DM:(r + 1) * DM, t0:t0 + 512].rearrange("(c p) t -> p c t", p=128), [h3_g], [h_], h_)
                    if t0 == 0:
                        k.dma(sp, gr[:], gt_g.t[bass.ds(k.pid_sp + r * NEXP, 1), :], [gt_g], [gr], gr)
                    k.op(pe, lambda: nc.tensor.matmul(gps[:], lhsT=ones1[0:1, :], rhs=gr[:, t0:t0 + 512], start=True, stop=True), reads=[ones1, gr], writes=[gps])
                    k.op(act, lambda: nc.scalar.copy(out=gb[:], in_=gps[:]), reads=[gps], writes=[gb])
                    for g0 in range(0, DEXP, 256):
                        wb = wpool[wcnt[0] % 2]
                        wcnt[0] += 1
                        k.dma(sp, wb[:, 0:KC, :], wegb.t[:, g0:g0 + 256].rearrange("(c p) n -> p c n", p=128), [wegb], [wb], wb)
                        k.dma(sp, wb[:, KC:2 * KC, :], weub.t[:, g0:g0 + 256].rearrange("(c p) n -> p c n", p=128), [weub], [wb], wb)
                        for j in range(2):
                            fc = g0 // 128 + j
                            pg = pps[pcnt[0] % 6]
                            pu = pps[(pcnt[0] + 1) % 6]
                            pcnt[0] += 2
                            k.op(pe, [(lambda kc=kc, j=j, pg=pg, wb=wb: nc.tensor.matmul(pg[:], lhsT=wb[:, kc, j * 128:(j + 1) * 128], rhs=h_[:, kc, :],
                                                                                         start=(kc == 0), stop=(kc == KC - 1))) for kc in range(KC)],
                                 reads=[wb, h_], writes=[pg])
                            k.op(pe, [(lambda kc=kc, j=j, pu=pu, wb=wb: nc.tensor.matmul(pu[:], lhsT=wb[:, KC + kc, j * 128:(j + 1) * 128], rhs=h_[:, kc, :],
                                                                                         start=(kc == 0), stop=(kc == KC - 1))) for kc in range(KC)],
                                 reads=[wb, h_], writes=[pu])
                            sg_ = sg[fc % 3]
                            k.op(act, lambda pg=pg, sg_=sg_: nc.scalar.activation(out=sg_[:], in_=pg[:], func=AF.Silu), reads=[pg], writes=[sg_])
                            k.op(dve, lambda pu=pu, sg_=sg_: nc.vector.tensor_tensor(out=sg_[:], in0=pu[:], in1=sg_[:], op=ALU.mult), reads=[pu, sg_], writes=[sg_])
                            k.op(dve, lambda fc=fc, sg_=sg_: nc.vector.tensor_tensor(out=actb[:, fc, :], in0=sg_[:], in1=gb[:], op=ALU.mult), reads=[sg_, gb], writes=[actb])
                    for g0 in range(0, DM, 256):
                        wb = wpool[wcnt[0] % 2]
                        wcnt[0] += 1
                        for c0 in range(0, NFC, 14):
                            c1 = min(NFC, c0 + 14)
                            k.dma(sp, wb[:, c0:c1, :], wedb.t[c0 * 128:c1 * 128, g0:g0 + 256].rearrange("(c p) n -> p c n", p=128), [wedb], [wb], wb)
                        for j in range(2):
                            pb = pps[pcnt[0] % 6]
                            pcnt[0] += 1
                            oc = g0 // 128 + j
                            k.op(pe, [(lambda fc=fc, j=j, pb=pb, wb=wb: nc.tensor.matmul(pb[:], lhsT=wb[:, fc, j * 128:(j + 1) * 128], rhs=actb[:, fc, :],
                                                                                         start=(fc == 0), stop=(fc == NFC - 1))) for fc in range(NFC)],
                                 reads=[wb, actb], writes=[pb])
                            y_ = ysb[ycnt[0] % 3]
                            ycnt[0] += 1
                            k.op(act, lambda pb=pb, y_=y_: nc.scalar.copy(out=y_[:], in_=pb[:]), reads=[pb], writes=[y_])
                            k.dma(sp, y_in[r].t[oc * 128:(oc + 1) * 128, t0:t0 + 512], y_[:], [y_], [y_in[r]], y_)
                    if t0 + 512 == TB:
                        k.coll("AllReduce", y_in[r], y_rr[r])
                        k.dma(pool, y_rb[r].t, y_rr[r].t, [y_rr[r]], [y_rb[r]], y_rb[r])
            with k.scope() as s:
                xt = [k.sb(f"fx{j}", [128, KC, 512], F32, s) for j in range(2)]
                yt = [k.sb(f"fy{j}", [128, KC, 512], F32, s) for j in range(2)]
                yview = y_r.t.rearrange("(r c p) t -> p (r c) t", r=NCORES, p=128)
                for sbk in range(NSB):
                    x_, y_ = xt[sbk % 2], yt[sbk % 2]
                    tsl = slice(sbk * 512, (sbk + 1) * 512)
                    k.dma(sp, x_[:], x3d.t[:, tsl].rearrange("(c p) t -> p c t", p=128), [x3d], [x_], x_)
                    k.dma(sp, y_[:], yview[:, bass.ds(k.pid_sp * KC, KC), tsl], y_rb, [y_], y_)
                    k.op(dve, lambda: nc.vector.tensor_tensor(out=x_[:], in0=x_[:], in1=y_[:], op=ALU.add), reads=[x_, y_], writes=[x_])
                    k.dma(sp, outT.t[:, tsl].rearrange("(c p) t -> p c t", p=128), x_[:], [x_], [outT], x_)

        phase_norm0()
        gather_cast(wg0s, [256, D_FF], wg0f, wg0b, "wg0")
        gather_cast(wu0s, [256, D_FF], wu0f, wu0b, "wu0")
        gather_cast(wd0s, [D_FF // 8, DM], wd0f, wd0b, "wd0")
        gather_cast(L[1]["wout"], [256, DM], wout_f[1], wout_bf[1], "wo1")
        cast_rows(weg, wegb, DM)
        cast_rows(weu, weub, DM)
        cast_rows(wed, wedb, DEXP)
        for i in range(2):
            if stop != 1.5:
                lam_setup(i)
        for i in range(2):
            if stop >= 2 + 2 * i:
                if stop != 2.2:
                    phase_diff(i)
                if stop != 2.1:
                    phase_dil(i)
            if stop >= 3 + 2 * i:
                phase_tok(i)
        if stop >= 6:
            phase_moe()
        for nm, (b, o) in k.dbg_out.items():
            k.dma(pool, o.t, b.t, [b], [o], o)
        if k.dbg_out:
            for nm, (b, o) in k.dbg_out.items():
                pool.eng.wait_ge(o.ds.sem, o.ds.n)
        k.finish_all()
    return k


def _consts(S):
    pos = np.arange(S, dtype=np.float32)

    def tab(dim):
        inv = (np.float32(ROPE_THETA) ** (-np.arange(0, dim, 2, dtype=np.float32) / np.float32(dim))).astype(np.float32)
        ang = (pos[None, :] * inv[:, None]).astype(np.float32)
        return np.cos(ang).astype(np.float32), np.sin(ang).astype(np.float32)
    ca, sa = tab(64)
    cbb, sbb = tab(128)

    def rot(d):
        R = np.zeros((d, d), np.float32)
        for j in range(d // 2):
            R[j + d // 2, j] = -1.0
            R[j, j + d // 2] = 1.0
        return R
    cf32 = np.zeros((128, 3, 128), np.float32)
    cf32[0:64, 0, 0:64] = rot(64)
    cf32[64:128, 0, 64:128] = rot(64)
    cf32[:, 1, :] = rot(128)
    cf32[:, 2, :] = np.eye(128, dtype=np.float32)
    ii = np.arange(128)[:, None]
    jj = np.arange(128)[None, :]
    cbf = np.zeros((128, 5, 128), np.float32)
    cbf[:, 0, :] = (jj <= ii)
    cbf[:, 1, :] = (jj >= ii)
    cbf[:, 2, :] = (jj <= ii) & (ii >= 64)
    cbf[:, 3, :] = (jj >= ii) & (ii < 64)
    cbf[:, 4, :] = np.eye(128)
    return dict(cosa=ca, sina=sa, cosb=cbb, sinb=sbb, cf32=cf32, cbf=cbf.astype(ml_dtypes.bfloat16))


def make_in_maps(inputs, S):
    TB = S // NCORES
    cs = _consts(S)
    x = np.asarray(inputs["x"])[0]
    maps = []
    for c in range(NCORES):
        m = dict(cs)
        m["xT"] = np.ascontiguousarray(x[c * TB:(c + 1) * TB, :].T)
        for i in range(2):
            m[f"an{i}"] = np.ascontiguousarray(np.asarray(inputs[f"attn_norm_{i}"]).reshape(KC, 128).T)
            m[f"fn{i}"] = np.ascontiguousarray(np.asarray(inputs[f"ffn_norm_{i}"]).reshape(KC, 128).T)
            w_in = np.asarray(inputs[f"w_in_{i}"])
            cols = np.concatenate([np.arange(b * 1024 + c * 128, b * 1024 + (c + 1) * 128) for b in range(6)])
            m[f"win{i}"] = np.ascontiguousarray(w_in[:, cols])
            g = np.zeros((128, 6), np.float32)
            g[:, 0] = np.tile(np.asarray(inputs[f"diff_q_norm_{i}"]), 2)
            g[:, 1] = np.tile(np.asarray(inputs[f"diff_k_norm_{i}"]), 2)
            g[:, 2] = np.asarray(inputs[f"dil_q_norm_{i}"])
            g[:, 3] = np.asarray(inputs[f"dil_k_norm_{i}"])
            g[:, 4] = np.asarray(inputs[f"diff_out_norm_{i}"])
            g[:, 5] = np.asarray(inputs[f"dil_out_norm_{i}"])
            m[f"gains{i}"] = g
            m[f"lamv{i}"] = np.ascontiguousarray(np.stack([np.asarray(inputs[f"diff_lam_{n}_{i}"]) for n in ("q1", "k1", "q2", "k2")], axis=1))
            m[f"wout{i}"] = np.ascontiguousarray(np.asarray(inputs[f"w_out_{i}"])[c * 256:(c + 1) * 256])
        m["wg0"] = np.ascontiguousarray(np.asarray(inputs["ffn_w_gate_0"])[c * 256:(c + 1) * 256])
        m["wu0"] = np.ascontiguousarray(np.asarray(inputs["ffn_w_up_0"])[c * 256:(c + 1) * 256])
        m["wd0"] = np.ascontiguousarray(np.asarray(inputs["ffn_w_down_0"])[c * 704:(c + 1) * 704])
        m["router"] = np.ascontiguousarray(np.asarray(inputs["router_1"]).reshape(KC, 128, NEXP).transpose(1, 0, 2))
        m["weg"] = np.ascontiguousarray(np.asarray(inputs["moe_w_gate_1"])[c])
        m["weu"] = np.ascontiguousarray(np.asarray(inputs["moe_w_up_1"])[c])
        m["wed"] = np.ascontiguousarray(np.asarray(inputs["moe_w_down_1"])[c])
        maps.append(m)
    return maps


_CACHE = {}


def run(inputs, S, debug=False, stop=99, lite=False):
    key = (S, debug, stop, lite)
    if key not in _CACHE:
        _CACHE[key] = build(S, debug, stop, lite)
    kk = _CACHE[key]
    maps = make_in_maps(inputs, S)
    if lite:
        for m in maps:
            for nm in ("weg", "weu", "wed"):
                m[nm] = np.ascontiguousarray(m[nm][:256, :] if nm == "wed" else m[nm][:, :256])
    res = run_bass_kernel_spmd(kk.nc, maps, core_ids=list(range(NCORES)))
    out = np.concatenate([np.asarray(r["outT"]).T for r in res.results], axis=0)[None]
    return out.astype(np.float32), res


def kernel(**inputs):
    S = int(np.asarray(inputs["x"]).shape[1])
    out, _ = run(inputs, S)
    return out
```

```python
import math
from contextlib import ExitStack

import numpy as np
import ml_dtypes
import concourse.bass as bass
import concourse.mybir as mybir
from concourse.bass_utils import run_bass_kernel_spmd

F32 = mybir.dt.float32
BF16 = mybir.dt.bfloat16
ALU = mybir.AluOpType
AF = mybir.ActivationFunctionType
AX = mybir.AxisListType

NCORES = 8
DM = 2048
KC = DM // 128
D_FF = 5632
D_EXP = 7168
NEXP = 8
EPS = 1e-6
ROPE_THETA = 10000.0
RG = [list(range(NCORES))]


class Prod:
    def __init__(self, sem, name):
        self.sem = sem
        self.n = 0
        self.name = name


class Buf:
    def __init__(self, t, name, partial=False):
        self.t = t
        self.name = name
        self.partial = partial
        self.w = {}
        self.r = {}
        self.ds = None

    def __getitem__(self, idx):
        return self.t[idx]


class Eng:
    def __init__(self, eng, prod):
        self.eng = eng
        self.prod = prod
        self.seen = {}


class K:
    def __init__(self, S, debug=False):
        self.S = S
        self.TB = S // NCORES
        self.NSB = self.TB // 512
        self.debug = debug
        self.nc = bass.Bass("TRN2", target_bir_lowering=False)
        self.st = ExitStack()
        self.nsem = 0
        self.dbg_out = {}
        self._all_bufs = []
        self.free_ds = []

    def sem(self, name):
        self.nsem += 1
        return self.st.enter_context(self.nc.semaphore(name))

    def mk_engines(self):
        nc = self.nc
        self.pe = Eng(nc.tensor, Prod(self.sem("s_pe"), "pe"))
        self.act = Eng(nc.scalar, Prod(self.sem("s_act"), "act"))
        self.dve = Eng(nc.vector, Prod(self.sem("s_dve"), "dve"))
        self.sp = Eng(nc.sync, Prod(None, "sp"))
        self.pool = Eng(nc.gpsimd, Prod(self.sem("s_pool"), "pool"))
        self.pid_sp = nc.sync.partition_id()
        self.pid_pool = nc.gpsimd.partition_id()

    def mkbuf(self, t, name, partial=False):
        b = Buf(t, name, partial)
        self._all_bufs.append(b)
        return b

    def _recycle(self, b):
        if b.ds is not None:
            b.ds.floor = b.ds.n
            self.free_ds.append(b.ds)
            b.ds = None

    def sb(self, name, shape, dt, stack=None, partial=False):
        st = stack if stack is not None else self.st
        self.uid = getattr(self, "uid", 0) + 1
        t = st.enter_context(self.nc.sbuf_tensor(f"{name}_{self.uid}", list(shape), dt))
        b = self.mkbuf(t, name, partial)
        b.is_sb = True
        if stack is not None:
            stack.callback(self._recycle, b)
        return b

    def ps(self, name, stack, shape=(128, 512), dt=F32):
        self.uid = getattr(self, "uid", 0) + 1
        full = [128, 512] if dt == F32 else [128, 1024]
        t = stack.enter_context(self.nc.psum_tensor(f"{name}_{self.uid}", full, dt))
        shape = tuple(shape)
        if len(shape) == 2:
            view = t[0:shape[0], 0:shape[1]]
        else:
            view = t[0:shape[0], 0:shape[1] * shape[2]].rearrange("p (a b) -> p a b", a=shape[1])
        return self.mkbuf(view, name)

    def ps_split(self, name, stack, n, w, dt):
        return [self.ps(f"{name}{j}", stack, (128, w), dt) for j in range(n)]

    def dram(self, name, shape, dt, kind="Internal"):
        t = self.nc.dram_tensor(name, list(shape), dt, kind=kind)
        return self.mkbuf(t.ap(), name, partial=True)

    def scope(self):
        st = ExitStack()
        st.callback(self.barrier)
        return st

    def barrier(self):
        prods = [self.pe.prod, self.act.prod, self.dve.prod]
        seen_ids = set()
        for b in self._all_bufs:
            if getattr(b, "is_sb", False) and b.ds is not None and id(b.ds) not in seen_ids:
                seen_ids.add(id(b.ds))
                prods.append(b.ds)
        for p in self.free_ds:
            if id(p) not in seen_ids:
                seen_ids.add(id(p))
                prods.append(p)
        for E in (self.pe, self.act, self.dve, self.sp):
            for p in prods:
                if p is not E.prod and p.n > 0 and E.seen.get(p, 0) < p.n:
                    E.eng.wait_ge(p.sem, p.n)
                    E.seen[p] = p.n

    def get_ds(self, sig, pre, fresh=False):
        if sig.ds is None:
            if self.free_ds and not fresh:
                sig.ds = self.free_ds.pop()
            else:
                sig.ds = Prod(self.sem(pre + sig.name), pre + sig.name)
                sig.ds.floor = 0
        return sig.ds

    def finish_all(self):
        nc = self.nc
        prods = set()
        for b in self._all_bufs:
            if b.ds is not None:
                prods.add(b.ds)
        for p in list(prods) + self.free_ds + [self.pe.prod, self.act.prod, self.dve.prod]:
            if p.n > 0 and self.sp.seen.get(p, 0) < p.n:
                nc.sync.wait_ge(p.sem, p.n)
                self.sp.seen[p] = p.n

    def _waits(self, E, reads, writes, ss=False):
        need = {}
        for b in reads:
            for p, c in b.w.items():
                if need.get(p, 0) < c:
                    need[p] = c
        for b in writes:
            if not b.partial:
                for p, c in b.w.items():
                    if need.get(p, 0) < c:
                        need[p] = c
            for p, c in b.r.items():
                if need.get(p, 0) < c:
                    need[p] = c
        for p, c in need.items():
            if p is E.prod and not ss:
                continue
            if E.seen.get(p, 0) < c:
                E.eng.wait_ge(p.sem, c)
                E.seen[p] = c

    def _record(self, prod, cnt, reads, writes):
        for b in reads:
            if b.r.get(prod, 0) < cnt:
                b.r[prod] = cnt
        for b in writes:
            if b.partial:
                if b.w.get(prod, 0) < cnt:
                    b.w[prod] = cnt
            else:
                b.w = {prod: cnt}
                b.r = {}

    def op(self, E, fns, reads=(), writes=(), ss=False):
        if not isinstance(fns, (list, tuple)):
            fns = [fns]
        self._waits(E, reads, writes, ss)
        ins = None
        for f in fns:
            ins = f()
        ins.then_inc(E.prod.sem, 1)
        E.prod.n += 1
        self._record(E.prod, E.prod.n, reads, writes)

    def dma(self, Q, out_ap, in_ap, reads, writes, sig):
        self._waits(Q, reads, writes)
        ds = self.get_ds(sig, "d_", fresh=(Q is self.pool))
        if Q.seen.get(ds, 0) < ds.floor:
            Q.eng.wait_ge(ds.sem, ds.floor)
            Q.seen[ds] = ds.floor
        Q.eng.dma_start(out=out_ap, in_=in_ap).then_inc(sig.ds.sem, 16)
        sig.ds.n += 16
        self._record(sig.ds, sig.ds.n, reads, writes)

    def coll(self, kind, src, dst):
        Q = self.pool
        self._waits(Q, [src], [dst])
        if dst.ds is None:
            dst.ds = Prod(self.sem("c_" + dst.name), "c_" + dst.name)
            dst.ds.floor = 0
        op = ALU.add if kind == "AllReduce" else ALU.bypass
        Q.eng.collective_compute(kind, op, replica_groups=RG, ins=[src.t.opt()], outs=[dst.t.opt()]).then_inc(dst.ds.sem)
        dst.ds.n += 1
        self._record(dst.ds, dst.ds.n, [src], [dst])

    def rstd_from_ms(self, ms_ps, rs, n=512):
        self.op(self.act, lambda: self.nc.scalar.activation(out=rs[:, 0:n], in_=ms_ps[:, 0:n], func=AF.Sqrt, bias=self.eps_t[:, 0:1], scale=1.0),
                reads=[ms_ps, self.eps_b], writes=[rs])
        self.op(self.dve, lambda: self.nc.vector.reciprocal(out=rs[:, 0:n], in_=rs[:, 0:n]), reads=[rs], writes=[rs])


def build(S, debug=False, stop=99, lite=False):
    k = K(S, debug)
    DEXP = 256 if lite else D_EXP
    nc = k.nc
    TB, NSB = k.TB, k.NSB
    NQ = S // 512
    NKT = S // 128
    PAD = 1024
    with k.st:
        k.mk_engines()
        pe, act, dve, sp, pool = k.pe, k.act, k.dve, k.sp, k.pool

        def ext_in(name, shape, dt=F32):
            return k.dram(name, shape, dt, kind="ExternalInput")

        xT = ext_in("xT", [DM, TB])
        cf32 = ext_in("cf32", [128, 3, 128])
        cbf = ext_in("cbf", [128, 5, 128], BF16)
        cosa = ext_in("cosa", [32, S])
        sina = ext_in("sina", [32, S])
        cosb = ext_in("cosb", [64, S])
        sinb = ext_in("sinb", [64, S])
        L = []
        for i in range(2):
            d = dict(
                an=ext_in(f"an{i}", [128, KC]), fn=ext_in(f"fn{i}", [128, KC]),
                win=ext_in(f"win{i}", [DM, 768]), gains=ext_in(f"gains{i}", [128, 6]),
                lamv=ext_in(f"lamv{i}", [64, 4]), wout=ext_in(f"wout{i}", [256, DM]),
            )
            L.append(d)
        wg0s = ext_in("wg0", [256, D_FF])
        wu0s = ext_in("wu0", [256, D_FF])
        wd0s = ext_in("wd0", [D_FF // 8, DM])
        router = ext_in("router", [128, KC, NEXP])
        weg = ext_in("weg", [DM, DEXP])
        weu = ext_in("weu", [DM, DEXP])
        wed = ext_in("wed", [DEXP, DM])
        outT = k.dram("outT", [DM, TB], F32, kind="ExternalOutput")

        def scratch(name, shape, dt):
            b = k.dram(name, shape, dt)
            if debug:
                k.dbg_out[name] = (b, k.dram("dbg_" + name, shape, dt, kind="ExternalOutput"))
            return b

        h_in = [k.dram(f"h_in{i}", [DM, TB], BF16) for i in range(2)]
        h_g = [k.dram(f"h_g{i}", [NCORES * DM, TB], BF16) for i in range(2)]
        a_in = [k.dram(f"a_in{i}", [256, S], BF16) for i in range(2)]
        a_g = [scratch(f"a_g{i}", [NCORES * 256, S], BF16) for i in range(2)]
        x1d = scratch("x1d", [DM, TB], F32)
        x3d = scratch("x3d", [DM, TB], F32)
        h3_in = k.dram("h3_in", [DM, TB], BF16)
        h3_g = k.dram("h3_g", [NCORES * DM, TB], BF16)
        gt_in = k.dram("gt_in", [NEXP, TB], F32)
        gt_g = scratch("gt_g", [NCORES * NEXP, TB], F32)
        y_in = [k.dram(f"y_in{r}", [DM, TB], F32) for r in range(NCORES)]
        y_r = k.dram("y_r", [NCORES * DM, TB], F32)
        y_rr = [k.dram(f"y_rr{r}", [DM, TB], F32) for r in range(NCORES)]
        y_rb = [k.mkbuf(y_r.t[r * DM:(r + 1) * DM, :], f"y_r{r}", partial=True) for r in range(NCORES)]
        wsh = [None, None]
        win_bf = [k.dram(f"win_bf{i}", [DM, 768], BF16) for i in range(2)]
        wout_f = [k.dram(f"wout_f{i}", [DM, DM], F32) for i in range(2)]
        wout_bf = [k.dram(f"wout_bf{i}", [DM, DM], BF16) for i in range(2)]
        wg0f = k.dram("wg0f", [DM, D_FF], F32)
        wu0f = k.dram("wu0f", [DM, D_FF], F32)
        wd0f = k.dram("wd0f", [D_FF, DM], F32)
        wg0b = k.dram("wg0b", [DM, D_FF], BF16)
        wu0b = k.dram("wu0b", [DM, D_FF], BF16)
        wd0b = k.dram("wd0b", [D_FF, DM], BF16)
        wegb = k.dram("wegb", [DM, DEXP], BF16)
        weub = k.dram("weub", [DM, DEXP], BF16)
        wedb = k.dram("wedb", [DEXP, DM], BF16)

        c32 = k.sb("c32", [128, 3, 128], F32)
        cb = k.sb("cb", [128, 5, 128], BF16)
        onesA = k.sb("onesA", [128, 128], F32)
        onesB = k.sb("onesB", [128, 128], F32)
        onesD = k.sb("onesD", [128, 128], F32)
        ones1 = k.sb("ones1", [128, 128], F32)
        onesb = k.sb("onesb", [128, 128], BF16)
        k.eps_b = k.sb("eps", [128, 1], F32)
        k.eps_t = k.eps_b.t
        nrm = [dict(an=k.sb(f"an_s{i}", [128, KC], F32), fn=k.sb(f"fn_s{i}", [128, KC], F32),
                    gains=k.sb(f"gains_s{i}", [128, 6], F32), lamv=k.sb(f"lamv_s{i}", [64, 4], F32),
                    nlam=k.sb(f"nlam{i}", [128, 1], F32), dog=k.sb(f"dog{i}", [128, 1], F32)) for i in range(2)]
        rt_s = k.sb("rt_s", [128, KC, NEXP], F32)

        k.dma(sp, c32[:], cf32.t, [cf32], [c32], c32)
        k.dma(sp, cb[:], cbf.t, [cbf], [cb], cb)
        k.dma(sp, rt_s[:], router.t, [router], [rt_s], rt_s)
        for i in range(2):
            for nm in ("an", "fn", "gains", "lamv"):
                k.dma(sp, nrm[i][nm][:], L[i][nm].t, [L[i][nm]], [nrm[i][nm]], nrm[i][nm])
        k.op(dve, lambda: nc.vector.memset(onesA[:], 0.0), writes=[onesA])
        k.op(dve, [lambda: nc.vector.memset(onesA[0:64, 0:64], 1.0 / 64), lambda: nc.vector.memset(onesA[64:128, 64:128], 1.0 / 64)], writes=[onesA], ss=True)
        k.op(dve, lambda: nc.vector.memset(onesB[:], 1.0 / 128), writes=[onesB])
        k.op(dve, lambda: nc.vector.memset(onesD[:], 1.0 / DM), writes=[onesD])
        k.op(dve, lambda: nc.vector.memset(ones1[:], 1.0), writes=[ones1])
        k.op(dve, lambda: nc.vector.memset(onesb[:], 1.0), writes=[onesb])
        k.op(dve, lambda: nc.vector.memset(k.eps_t[:], EPS), writes=[k.eps_b])
        rotA, rotB, ident32 = 0, 1, 2
        M_LO, M_HI, M_LO0, M_HIL, IDB = 0, 1, 2, 3, 4

        def cast_rows(src, dst, rows, blk=256):
            for r0 in range(0, rows, blk):
                r1 = min(rows, r0 + blk)
                k.dma(pool, dst.t[r0:r1, :], src.t[r0:r1, :], [src], [dst], dst)
        for i in range(2):
            cast_rows(L[i]["win"], win_bf[i], DM)
        def gather_cast(src_ext, shard_shape, full_f, full_b, nm):
            bounce = k.dram("bnc_" + nm, shard_shape, F32)
            k.dma(pool, bounce.t, src_ext.t, [src_ext], [bounce], bounce)
            k.coll("AllGather", bounce, full_f)
            cast_rows(full_f, full_b, shard_shape[0] * NCORES)
        gather_cast(L[0]["wout"], [256, DM], wout_f[0], wout_bf[0], "wo0")

        def rmsnorm_tile(xt, g_s, hb, stack_ps, pools, hf_cb=None):
            ms = pools["ms_ps"]
            for kc in range(KC):
                sq = pools["sq"][kc % 2]
                k.op(act, lambda kc=kc, sq=sq: nc.scalar.activation(out=sq[:], in_=xt[:, kc, :], func=AF.Square), reads=[xt], writes=[sq])
                k.op(pe, lambda kc=kc, sq=sq: nc.tensor.matmul(ms[:], lhsT=onesD[:], rhs=sq[:], start=(kc == 0), stop=(kc == KC - 1)),
                     reads=[sq, onesD], writes=[ms])
            rs = pools["rs"]
            k.rstd_from_ms(ms, rs)
            for kc in range(KC):
                if hf_cb is None:
                    k.op(dve, lambda kc=kc: nc.vector.scalar_tensor_tensor(out=hb[:, kc, :], in0=xt[:, kc, :], scalar=g_s[:, kc:kc + 1], in1=rs[:],
                                                                          op0=ALU.mult, op1=ALU.mult), reads=[xt, g_s, rs], writes=[hb])
                else:
                    hf = pools["sq"][kc % 2]
                    k.op(dve, lambda kc=kc, hf=hf: nc.vector.scalar_tensor_tensor(out=hf[:], in0=xt[:, kc, :], scalar=g_s[:, kc:kc + 1], in1=rs[:],
                                                                                op0=ALU.mult, op1=ALU.mult), reads=[xt, g_s, rs], writes=[hf])
                    k.op(act, lambda kc=kc, hf=hf: nc.scalar.copy(out=hb[:, kc, :], in_=hf[:]), reads=[hf], writes=[hb])
                    hf_cb(kc, hf)

        def phase_norm0():
            with k.scope() as s:
                xt = [k.sb(f"n0x{j}", [128, KC, 512], F32, s) for j in range(2)]
                hb = [k.sb(f"n0h{j}", [128, KC, 512], BF16, s) for j in range(2)]
                pools = dict(sq=[k.sb(f"n0sq{j}", [128, 512], F32, s) for j in range(2)], rs=k.sb("n0rs", [128, 512], F32, s),
                             ms_ps=k.ps("n0ms", s))
                for sbk in range(NSB):
                    x_, h_ = xt[sbk % 2], hb[sbk % 2]
                    k.dma(sp, x_[:], xT.t[:, sbk * 512:(sbk + 1) * 512].rearrange("(c p) t -> p c t", p=128), [xT], [x_], x_)
                    rmsnorm_tile(x_, nrm[0]["an"], h_, s, pools)
                    k.dma(sp, h_in[0].t[:, sbk * 512:(sbk + 1) * 512].rearrange("(c p) t -> p c t", p=128), h_[:], [h_], [h_in[0]], h_)
            k.coll("AllGather", h_in[0], h_g[0])

        def lam_setup(i):
            lam_init = 0.8 - 0.6 * math.exp(-0.3 * i)
            with k.scope() as s:
                pr = k.sb("lampr", [64, 2], F32, s)
                ee = k.sb("lamee", [128, 2], F32, s)
                lp = k.ps("lamps", s, (128, 2))
                lv = nrm[i]["lamv"]
                k.op(dve, [lambda: nc.vector.tensor_tensor(out=pr[:, 0:1], in0=lv[:, 0:1], in1=lv[:, 1:2], op=ALU.mult),
                           lambda: nc.vector.tensor_tensor(out=pr[:, 1:2], in0=lv[:, 2:3], in1=lv[:, 3:4], op=ALU.mult)], reads=[lv], writes=[pr])
                k.op(pe, lambda: nc.tensor.matmul(lp[:], lhsT=ones1[0:64, :], rhs=pr[:], start=True, stop=True), reads=[pr, ones1], writes=[lp])
                k.op(act, lambda: nc.scalar.activation(out=ee[:], in_=lp[:], func=AF.Exp), reads=[lp], writes=[ee])
                nl = nrm[i]["nlam"]
                k.op(dve, lambda: nc.vector.tensor_tensor(out=nl[:], in0=ee[:, 1:2], in1=ee[:, 0:1], op=ALU.subtract), reads=[ee], writes=[nl])
                k.op(dve, lambda: nc.vector.tensor_scalar(out=nl[:], in0=nl[:], scalar1=-lam_init, scalar2=None, op0=ALU.add), reads=[nl], writes=[nl], ss=True)
                k.op(dve, lambda: nc.vector.tensor_scalar(out=nrm[i]["dog"][:], in0=nrm[i]["gains"][:, 4:5], scalar1=(1.0 - lam_init), scalar2=None, op0=ALU.mult),
                     reads=[nrm[i]["gains"]], writes=[nrm[i]["dog"]])

        def project(i, s, col0, ncol_chunks, dests, wsb, hpool, post):
            pp = [k.ps(f"prj{j}", s) for j in range(2)]
            cnt = 0
            for qi in range(NQ):
                r, t0 = divmod(qi * 512, TB)
                hb = hpool[qi % 2]
                k.dma(sp, hb[:], h_g[i].t[r * DM:(r + 1) * DM, t0:t0 + 512].rearrange("(c p) t -> p c t", p=128), [h_g[i]], [hb], hb)
                for j in range(ncol_chunks):
                    pb = pp[cnt % 2]
                    cnt += 1
                    c0 = col0 + j * 128
                    k.op(pe, [(lambda kc=kc, pb=pb, c0=c0: nc.tensor.matmul(pb[:], lhsT=wsb[:, kc, c0:c0 + 128], rhs=hb[:, kc, :], start=(kc == 0), stop=(kc == KC - 1)))
                              for kc in range(KC)], reads=[wsb, hb], writes=[pb])
                    post(j, qi, pb)

        def load_w_in(i, s):
            wsb = k.sb("winsb", [128, KC, 768], BF16, s)
            k.dma(sp, wsb[:], win_bf[i].t.rearrange("(c p) n -> p c n", p=128), [win_bf[i]], [wsb], wsb)
            return wsb

        def out_norm_store(s, o32, gain_ap, gain_buf, row0, i, qcols, pools):
            sq, ax, rs, ob = pools["sq"], pools["ax"], pools["rs"], pools["ob"]
            k.op(act, lambda: nc.scalar.activation(out=sq[:], in_=o32[:], func=AF.Square), reads=[o32], writes=[sq])
            k.op(pe, lambda: nc.tensor.matmul(ax[:], lhsT=onesB[:], rhs=sq[:], start=True, stop=True), reads=[onesB, sq], writes=[ax])
            k.rstd_from_ms(ax, rs)
            k.op(dve, lambda: nc.vector.scalar_tensor_tensor(out=ob[:], in0=o32[:], scalar=gain_ap, in1=rs[:], op0=ALU.mult, op1=ALU.mult),
                 reads=[o32, gain_buf, rs], writes=[ob])
            k.dma(sp, a_in[i].t[row0:row0 + 128, qcols], ob[:], [ob], [a_in[i]], ob)

        def load_cs(qi, cpool, is_a):
            cs = cpool[qi % 2]
            c_, s_ = cs
            sl = slice(qi * 512, (qi + 1) * 512)
            if is_a:
                for g in range(4):
                    k.dma(sp, c_[g * 32:(g + 1) * 32, :], cosa.t[:, sl], [cosa], [c_], c_)
                    k.dma(sp, s_[g * 32:(g + 1) * 32, :], sina.t[:, sl], [sina], [s_], s_)
            else:
                for g in range(2):
                    k.dma(sp, c_[g * 64:(g + 1) * 64, :], cosb.t[:, sl], [cosb], [c_], c_)
                    k.dma(sp, s_[g * 64:(g + 1) * 64, :], sinb.t[:, sl], [sinb], [s_], s_)
            return cs

        def phase_diff(i):
            with k.scope() as s:
                qaT = k.sb("qaT", [128, S], BF16, s, partial=True)
                kaT = k.sb("kaT", [128, S], BF16, s, partial=True)
                vtok = k.sb("vtok", [128, NKT, 128], BF16, s, partial=True)
                with k.scope() as s2:
                    wsb = load_w_in(i, s2)
                    hpool = [k.sb(f"dh{j}", [128, KC, 512], BF16, s2) for j in range(2)]
                    cpool = [(k.sb(f"dc{j}", [128, 512], F32, s2, partial=True), k.sb(f"dsn{j}", [128, 512], F32, s2, partial=True)) for j in range(2)]
                    vch = [k.sb(f"dvch{j}", [128, 512], BF16, s2) for j in range(2)]
                    tp = k.ps_split("vtp", s2, 2, 128, BF16)
                    ws_d = mk_qk_ws(s2, "da")
                    cur = {}

                    def post(j, qi, pb):
                        if j == 0:
                            cur["cs"] = load_cs(qi, cpool, True)
                        if j < 2:
                            _qk(j, qi, pb, (qaT, kaT)[j], 0, onesA, rotA, cur["cs"], i, ws_d)
                        else:
                            vc = vch[qi % 2]
                            k.op(act, lambda: nc.scalar.copy(out=vc[:], in_=pb[:]), reads=[pb], writes=[vc])
                            for t in range(4):
                                tb = tp[t % 2]
                                k.op(pe, lambda t=t, tb=tb: nc.tensor.transpose(tb[:], vc[:, t * 128:(t + 1) * 128], cb[:, IDB, :]), reads=[vc, cb], writes=[tb])
                                k.op(dve, lambda t=t, tb=tb: nc.vector.tensor_copy(out=vtok[:, qi * 4 + t, :], in_=tb[:]), reads=[tb], writes=[vtok])
                    project(i, s2, 0, 3, None, wsb, hpool, post)
                with k.scope() as s3:
                    kz = [k.sb(f"kz{m}", [128, S], BF16, s3) for m in range(2)]
                    for c0 in range(0, S, 2048):
                        cs_ = slice(c0, c0 + 2048)
                        k.op(dve, [lambda: nc.vector.memset(kz[0][64:128, cs_], 0.0), lambda: nc.vector.tensor_copy(out=kz[0][0:64, cs_], in_=kaT[0:64, cs_]),
                                   lambda: nc.vector.memset(kz[1][0:64, cs_], 0.0), lambda: nc.vector.tensor_copy(out=kz[1][64:128, cs_], in_=kaT[64:128, cs_])],
                             reads=[kaT], writes=[kz[0], kz[1]])
                    sps = [[k.ps(f"ss{m}{j}", s3) for j in range(2)] for m in range(2)]
                    ops_ = [k.ps(f"oo{m}", s3) for m in range(2)]
                    dps = [k.ps(f"dd{m}", s3) for m in range(2)]
                    pts = [[k.sb(f"pt{m}{j}", [128, 512], BF16, s3) for j in range(3)] for m in range(2)]
                    r_ = [k.sb(f"dr{m}", [128, 512], F32, s3) for m in range(2)]
                    ab = [k.sb(f"dab{m}", [128, 512], F32, s3) for m in range(2)]
                    o32 = k.sb("do32", [128, 512], F32, s3)
                    pools = dict(sq=k.sb("dosq", [128, 512], F32, s3), rs=k.sb("dors", [128, 512], F32, s3), ob=k.sb("doob", [128, 512], BF16, s3))
                    step = 0
                    for qi in range(NQ):
                        qs = slice(qi * 512, (qi + 1) * 512)

                        def emit_S1(kt, bi, m):
                            ks = slice(kt * 128, (kt + 1) * 128)
                            sp_ = sps[m][bi]
                            k.op(pe, lambda: nc.tensor.matmul(sp_[:], lhsT=kz[m][:, ks], rhs=qaT[:, qs], start=True, stop=True),
                                 reads=[kz[m], qaT], writes=[sp_])
                        emit_S1(0, 0, 0)
                        emit_S1(0, 0, 1)
                        for kt in range(NKT):
                            bi = kt % 2
                            ptk = [pts[m][kt % 3] for m in range(2)]
                            for m in range(2):
                                k.op(act, lambda m=m: nc.scalar.activation(out=ptk[m][:], in_=sps[m][bi][:], func=AF.Exp, scale=0.125), reads=[sps[m][bi]], writes=[ptk[m]])
                            for m in range(2):
                                if kt + 1 < NKT:
                                    emit_S1(kt + 1, 1 - bi, m)
                                pt = ptk[m]
                                k.op(pe, [lambda pt=pt, m=m, kt=kt: nc.tensor.matmul(ops_[m][:], lhsT=vtok[:, kt, :], rhs=pt[:], start=(kt == 0), stop=(kt == NKT - 1)),
                                          lambda pt=pt, m=m, kt=kt: nc.tensor.matmul(dps[m][:], lhsT=onesb[:], rhs=pt[:], start=(kt == 0), stop=(kt == NKT - 1))],
                                     reads=[vtok, pt, onesb], writes=[ops_[m], dps[m]])
                            step += 1
                        for m in range(2):
                            k.op(dve, lambda m=m: nc.vector.reciprocal(out=r_[m][:], in_=dps[m][:]), reads=[dps[m]], writes=[r_[m]])
                            k.op(dve, lambda m=m: nc.vector.tensor_tensor(out=ab[m][:], in0=ops_[m][:], in1=r_[m][:], op=ALU.mult), reads=[ops_[m], r_[m]], writes=[ab[m]])
                        nl = nrm[i]["nlam"]
                        k.op(dve, lambda: nc.vector.scalar_tensor_tensor(out=o32[:], in0=ab[1][:], scalar=nl[:, 0:1], in1=ab[0][:], op0=ALU.mult, op1=ALU.add),
                             reads=[ab[0], ab[1], nl], writes=[o32])
                        pools["ax"] = sps[0][0]
                        out_norm_store(s3, o32, nrm[i]["dog"][:, 0:1], nrm[i]["dog"], 0, i, qs, pools)

        def mk_qk_ws(s, nm):
            return dict(sq=[k.sb(f"{nm}sq{j}", [128, 512], F32, s) for j in range(2)], qn=[k.sb(f"{nm}qn{j}", [128, 512], F32, s) for j in range(2)],
                        rs=[k.sb(f"{nm}rs{j}", [128, 512], F32, s) for j in range(2)], t1=[k.sb(f"{nm}t1{j}", [128, 512], F32, s) for j in range(2)],
                        aux=[k.ps(f"{nm}aux{j}", s) for j in range(2)], n=0)

        def _qk(gidx, qi, pb, dst, dst_off, ones_m, rot_idx, cs, i_layer, ws):
            n = ws["n"]
            ws["n"] += 1
            sq_, qn_, rs_, t1_, ax = ws["sq"][n % 2], ws["qn"][n % 2], ws["rs"][n % 2], ws["t1"][n % 2], ws["aux"][n % 2]
            g_s = nrm[i_layer]["gains"]
            k.op(act, lambda: nc.scalar.activation(out=sq_[:], in_=pb[:], func=AF.Square), reads=[pb], writes=[sq_])
            k.op(pe, lambda: nc.tensor.matmul(ax[:], lhsT=ones_m[:], rhs=sq_[:], start=True, stop=True), reads=[ones_m, sq_], writes=[ax])
            k.rstd_from_ms(ax, rs_)
            k.op(dve, lambda: nc.vector.scalar_tensor_tensor(out=qn_[:], in0=pb[:], scalar=g_s[:, gidx:gidx + 1], in1=rs_[:], op0=ALU.mult, op1=ALU.mult),
                 reads=[pb, g_s, rs_], writes=[qn_])
            k.op(pe, lambda: nc.tensor.matmul(ax[:], lhsT=c32[:, rot_idx, :], rhs=qn_[:], start=True, stop=True), reads=[c32, qn_], writes=[ax])
            cosb_, sinb_ = cs
            k.op(dve, lambda: nc.vector.tensor_tensor(out=t1_[:], in0=qn_[:], in1=cosb_[:], op=ALU.mult), reads=[qn_, cosb_], writes=[t1_])
            k.op(dve, lambda: nc.vector.tensor_tensor(out=qn_[:], in0=ax[:], in1=sinb_[:], op=ALU.mult), reads=[ax, sinb_], writes=[qn_])
            k.op(dve, lambda: nc.vector.tensor_tensor(out=dst[:, dst_off + qi * 512:dst_off + (qi + 1) * 512], in0=t1_[:], in1=qn_[:], op=ALU.add),
                 reads=[t1_, qn_], writes=[dst])

        def phase_dil(i):
            SP_ = S + 2 * PAD
            QR = min(S, 4096)
            with k.scope() as s:
                qbT = k.sb("qbT", [128, S], BF16, s, partial=True)
                kbT = k.sb("kbT", [128, SP_], BF16, s, partial=True)
                vbT = k.sb("vbT", [128, SP_], BF16, s, partial=True)
                k.op(dve, [lambda: nc.vector.memset(kbT[:, 0:PAD], 0.0), lambda: nc.vector.memset(kbT[:, PAD + S:SP_], 0.0),
                           lambda: nc.vector.memset(vbT[:, 0:PAD], 0.0), lambda: nc.vector.memset(vbT[:, PAD + S:SP_], 0.0)], writes=[kbT, vbT])
                with k.scope() as s2:
                    wsb = load_w_in(i, s2)
                    hpool = [k.sb(f"bh{j}", [128, KC, 512], BF16, s2) for j in range(2)]
                    cpool = [(k.sb(f"bc{j}", [128, 512], F32, s2, partial=True), k.sb(f"bsn{j}", [128, 512], F32, s2, partial=True)) for j in range(2)]
                    ws_b = mk_qk_ws(s2, "db")
                    cur = {}

                    def post(j, qi, pb):
                        if j == 0:
                            cur["cs"] = load_cs(qi, cpool, False)
                        if j == 0:
                            _qk(2, qi, pb, qbT, 0, onesB, rotB, cur["cs"], i, ws_b)
                        elif j == 1:
                            _qk(3, qi, pb, kbT, PAD, onesB, rotB, cur["cs"], i, ws_b)
                        else:
                            k.op(act, lambda: nc.scalar.copy(out=vbT[:, PAD + qi * 512:PAD + (qi + 1) * 512], in_=pb[:]), reads=[pb], writes=[vbT])
                    project(i, s2, 384, 3, None, wsb, hpool, post)
                with k.scope() as s3:
                    oacc = k.sb("oacc", [128, QR], F32, s3, partial=True)
                    dacc = k.sb("dacc", [128, QR], F32, s3, partial=True)
                    sps = k.ps_split("bs", s3, 2, 128, F32)
                    ops_ = k.ps_split("bo", s3, 2, 128, F32)
                    dps = k.ps_split("bd", s3, 2, 128, F32)
                    tps = k.ps_split("bt", s3, 1, 128, BF16)
                    pf = [k.sb(f"bpf{j}", [128, 128], BF16, s3) for j in range(3)]
                    pm = [k.sb(f"bpm{j}", [128, 128], BF16, s3) for j in range(3)]
                    vt = [k.sb(f"bvt{j}", [128, 128], BF16, s3) for j in range(3)]
                    o32 = k.sb("bo32", [128, 512], F32, s3)
                    rd = k.sb("brd", [128, 512], F32, s3)
                    pools = dict(sq=k.sb("bosq", [128, 512], F32, s3), rs=k.sb("bors", [128, 512], F32, s3), ob=k.sb("boob", [128, 512], BF16, s3),
                                 ax=k.ps("boax", s3))
                    scale = 128 ** -0.5
                    cnt = dict(q=0, v=0, p=0)
                    for t0 in range(0, S, QR):
                        first = True
                        for dil in (1, 4, 16):
                            Ld = S // dil
                            nqt = QR // dil // 128
                            for r in range(dil):
                                mbase = t0 // dil
                                vtiles = {}

                                def get_v(j, dil=dil, r=r, mbase=mbase, vtiles=vtiles):
                                    if j in vtiles:
                                        return vtiles[j]
                                    n0 = mbase + 128 * j - 64
                                    c0 = PAD + r + dil * n0
                                    tb = tps[0]
                                    vb = vt[cnt["v"] % 3]
                                    cnt["v"] += 1
                                    k.op(pe, lambda: nc.tensor.transpose(tb[:], vbT[:, c0:c0 + 127 * dil + 1:dil], cb[:, IDB, :]), reads=[vbT, cb], writes=[tb])
                                    k.op(dve, lambda: nc.vector.tensor_copy(out=vb[:], in_=tb[:]), reads=[tb], writes=[vb])
                                    vtiles[j] = (vb, c0)
                                    return vtiles[j]
                                for qt in range(nqt):
                                    m0 = mbase + 128 * qt
                                    qc0 = r + dil * m0
                                    ob_, db_ = ops_[cnt["q"] % 2], dps[cnt["q"] % 2]
                                    cnt["q"] += 1
                                    for half in range(2):
                                        vb, c0 = get_v(qt + half)
                                        if half == 0:
                                            mk = M_LO0 if m0 == 0 else M_LO
                                        else:
                                            mk = M_HIL if m0 == Ld - 128 else M_HI
                                        sp_ = sps[cnt["p"] % 2]
                                        pf_ = pf[cnt["p"] % 3]
                                        pm_ = pm[cnt["p"] % 3]
                                        cnt["p"] += 1
                                        k.op(pe, lambda sp_=sp_, c0=c0: nc.tensor.matmul(sp_[:], lhsT=kbT[:, c0:c0 + 127 * dil + 1:dil], rhs=qbT[:, qc0:qc0 + 127 * dil + 1:dil],
                                                                                          start=True, stop=True), reads=[kbT, qbT], writes=[sp_])
                                        k.op(act, lambda sp_=sp_, pf_=pf_: nc.scalar.activation(out=pf_[:], in_=sp_[:], func=AF.Exp, scale=scale), reads=[sp_], writes=[pf_])
                                        k.op(dve, lambda pf_=pf_, pm_=pm_, mk=mk: nc.vector.tensor_tensor(out=pm_[:], in0=pf_[:], in1=cb[:, mk, :], op=ALU.mult),
                                             reads=[pf_, cb], writes=[pm_])
                                        k.op(pe, [lambda vb=vb, pm_=pm_, half=half: nc.tensor.matmul(ob_[:], lhsT=vb[:], rhs=pm_[:], start=(half == 0), stop=(half == 1)),
                                                  lambda pm_=pm_, half=half: nc.tensor.matmul(db_[:], lhsT=onesb[:], rhs=pm_[:], start=(half == 0), stop=(half == 1))],
                                             reads=[vb, pm_, onesb], writes=[ob_, db_])
                                    lc0 = qc0 - t0
                                    osl = oacc[:, lc0:lc0 + 127 * dil + 1:dil]
                                    dsl = dacc[:, lc0:lc0 + 127 * dil + 1:dil]
                                    if first:
                                        k.op(dve, [lambda: nc.vector.tensor_copy(out=osl, in_=ob_[:]), lambda: nc.vector.tensor_copy(out=dsl, in_=db_[:])],
                                             reads=[ob_, db_], writes=[oacc, dacc])
                                    else:
                                        k.op(dve, [lambda: nc.vector.tensor_tensor(out=osl, in0=ob_[:], in1=osl, op=ALU.add),
                                                   lambda: nc.vector.tensor_tensor(out=dsl, in0=db_[:], in1=dsl, op=ALU.add)],
                                             reads=[ob_, db_, oacc, dacc], writes=[oacc, dacc])
                            first = False
                        for c in range(QR // 512):
                            cs_ = slice(c * 512, (c + 1) * 512)
                            k.op(dve, lambda: nc.vector.reciprocal(out=rd[:], in_=dacc[:, cs_]), reads=[dacc], writes=[rd])
                            k.op(dve, lambda: nc.vector.tensor_tensor(out=o32[:], in0=oacc[:, cs_], in1=rd[:], op=ALU.mult), reads=[oacc, rd], writes=[o32])
                            out_norm_store(s3, o32, nrm[i]["gains"][:, 5:6], nrm[i]["gains"], 128, i, slice(t0 + c * 512, t0 + (c + 1) * 512), pools)

        def stream_mm(s, wdram, KCH, ncols, wpool, wcnt, rhs_buf, rhs_reads, consume, pps, pcnt, group=256):
            for g0 in range(0, ncols, group):
                wb = wpool[wcnt[0] % len(wpool)]
                wcnt[0] += 1
                gw = min(group, ncols - g0)
                k.dma(sp, wb[:, 0:KCH, 0:gw], wdram.t[:, g0:g0 + gw].rearrange("(c p) n -> p c n", p=128), [wdram], [wb], wb)
                for j in range(gw // 128):
                    pb = pps[pcnt[0] % len(pps)]
                    pcnt[0] += 1
                    k.op(pe, [(lambda kc=kc, j=j, pb=pb, wb=wb: nc.tensor.matmul(pb[:], lhsT=wb[:, kc, j * 128:(j + 1) * 128], rhs=rhs_buf[:, kc, :],
                                                                                 start=(kc == 0), stop=(kc == KCH - 1))) for kc in range(KCH)],
                         reads=[wb] + rhs_reads, writes=[pb])
                    consume((g0 // 128) + j, pb)

        def phase_tok(i):
            k.coll("AllGather", a_in[i], a_g[i])
            x_src = xT if i == 0 else x1d
            with k.scope() as s:
                xt = [k.sb(f"tx{j}", [128, KC, 512], F32, s) for j in range(1)]
                asb = [k.sb(f"ta{j}", [128, KC, 512], BF16, s) for j in range(1)]
                h2 = k.sb("th2", [128, KC, 512], BF16, s)
                wpool = [k.sb(f"tw{j}", [128, 44, 256], BF16, s) for j in range(2)]
                pps = [k.ps(f"tp{j}", s) for j in range(4 if i == 0 else 2)]
                NPP = len(pps)
                npool = dict(sq=[k.sb(f"tsq{j}", [128, 512], F32, s) for j in range(2)], rs=k.sb("trs", [128, 512], F32, s), ms_ps=k.ps("tms", s))
                wcnt, pcnt = [0], [0]
                if i == 0:
                    actb = k.sb("tact", [128, D_FF // 128, 512], BF16, s)
                    sg = [k.sb(f"tsg{j}", [128, 512], F32, s) for j in range(2)]
                    hn = h2
                else:
                    lg_ps = [k.ps(f"tlg{tt}", s, (128, NEXP)) for tt in range(4)]
                    gT_ps = k.ps("tgt", s, (NEXP, 512))
                    lgs = k.sb("tlgs", [128, 4, NEXP], F32, s)
                    mx8 = k.sb("tmx8", [128, 4, 8], F32, s)
                    msk = k.sb("tmsk", [128, 4, NEXP], F32, s)
                    ee = k.sb("tee", [128, 4, NEXP], F32, s)
                    nv1 = k.sb("tnv1", [128, 4], F32, s)
                    den = k.sb("tden", [128, 4], F32, s)
                    gts = k.sb("tgts", [NEXP, 512], F32, s)
                for sbk in range(NSB):
                    x_, a_ = xt[0], asb[0]
                    tsl = slice(sbk * 512, (sbk + 1) * 512)
                    k.dma(sp, x_[:], x_src.t[:, tsl].rearrange("(c p) t -> p c t", p=128), [x_src], [x_], x_)
                    k.dma(sp, a_[:], a_g[i].t.rearrange("(c p) t -> p c t", p=128)[:, :, bass.ds(k.pid_sp * TB + sbk * 512, 512)], [a_g[i]], [a_], a_)
                    amap = [2 * r for r in range(8)] + [2 * r + 1 for r in range(8)]
                    for g0 in range(0, DM, 256):
                        wb = wpool[wcnt[0] % 2]
                        wcnt[0] += 1
                        k.dma(sp, wb[:, 0:KC, :], wout_bf[i].t[:, g0:g0 + 256].rearrange("(c p) n -> p c n", p=128), [wout_bf[i]], [wb], wb)
                        for j in range(2):
                            pb = pps[pcnt[0] % NPP]
                            pcnt[0] += 1
                            oc = g0 // 128 + j
                            k.op(pe, [(lambda kc=kc, j=j, pb=pb, wb=wb: nc.tensor.matmul(pb[:], lhsT=wb[:, kc, j * 128:(j + 1) * 128], rhs=a_[:, amap[kc], :],
                                                                                         start=(kc == 0), stop=(kc == KC - 1))) for kc in range(KC)],
                                 reads=[wb, a_], writes=[pb])
                            k.op(dve, lambda oc=oc, pb=pb: nc.vector.tensor_tensor(out=x_[:, oc, :], in0=pb[:], in1=x_[:, oc, :], op=ALU.add), reads=[pb, x_], writes=[x_])
                    if i == 0:
                        rmsnorm_tile(x_, nrm[0]["fn"], h2, s, npool)
                        for g0 in range(0, D_FF, 256):
                            wb = wpool[wcnt[0] % 2]
                            wcnt[0] += 1
                            k.dma(sp, wb[:, 0:KC, :], wg0b.t[:, g0:g0 + 256].rearrange("(c p) n -> p c n", p=128), [wg0b], [wb], wb)
                            k.dma(sp, wb[:, KC:2 * KC, :], wu0b.t[:, g0:g0 + 256].rearrange("(c p) n -> p c n", p=128), [wu0b], [wb], wb)
                            for j in range(2):
                                fc = g0 // 128 + j
                                pg = pps[pcnt[0] % NPP]
                                pu = pps[(pcnt[0] + 1) % NPP]
                                pcnt[0] += 2
                                k.op(pe, [(lambda kc=kc, j=j, pg=pg, wb=wb: nc.tensor.matmul(pg[:], lhsT=wb[:, kc, j * 128:(j + 1) * 128], rhs=h2[:, kc, :],
                                                                                             start=(kc == 0), stop=(kc == KC - 1))) for kc in range(KC)],
                                     reads=[wb, h2], writes=[pg])
                                k.op(pe, [(lambda kc=kc, j=j, pu=pu, wb=wb: nc.tensor.matmul(pu[:], lhsT=wb[:, KC + kc, j * 128:(j + 1) * 128], rhs=h2[:, kc, :],
                                                                                             start=(kc == 0), stop=(kc == KC - 1))) for kc in range(KC)],
                                     reads=[wb, h2], writes=[pu])
                                sg_ = sg[fc % 2]
                                k.op(act, lambda pg=pg, sg_=sg_: nc.scalar.activation(out=sg_[:], in_=pg[:], func=AF.Silu), reads=[pg], writes=[sg_])
                                k.op(dve, lambda fc=fc, pu=pu, sg_=sg_: nc.vector.tensor_tensor(out=actb[:, fc, :], in0=pu[:], in1=sg_[:], op=ALU.mult),
                                     reads=[pu, sg_], writes=[actb])
                        for g0 in range(0, DM, 256):
                            wb = wpool[wcnt[0] % 2]
                            wcnt[0] += 1
                            for c0 in range(0, 44, 11):
                                k.dma(sp, wb[:, c0:c0 + 11, :], wd0b.t[c0 * 128:(c0 + 11) * 128, g0:g0 + 256].rearrange("(c p) n -> p c n", p=128), [wd0b], [wb], wb)
                            for j in range(2):
                                pb = pps[pcnt[0] % NPP]
                                pcnt[0] += 1
                                oc = g0 // 128 + j
                                k.op(pe, [(lambda fc=fc, j=j, pb=pb, wb=wb: nc.tensor.matmul(pb[:], lhsT=wb[:, fc, j * 128:(j + 1) * 128], rhs=actb[:, fc, :],
                                                                                             start=(fc == 0), stop=(fc == 43))) for fc in range(44)],
                                     reads=[wb, actb], writes=[pb])
                                k.op(dve, lambda oc=oc, pb=pb: nc.vector.tensor_tensor(out=x_[:, oc, :], in0=pb[:], in1=x_[:, oc, :], op=ALU.add), reads=[pb, x_], writes=[x_])
                        k.dma(sp, x1d.t[:, tsl].rearrange("(c p) t -> p c t", p=128), x_[:], [x_], [x1d], x_)
                        rmsnorm_tile(x_, nrm[1]["an"], hn, s, npool)
                        k.dma(sp, h_in[1].t[:, tsl].rearrange("(c p) t -> p c t", p=128), hn[:], [hn], [h_in[1]], hn)
                    else:
                        k.dma(sp, x3d.t[:, tsl].rearrange("(c p) t -> p c t", p=128), x_[:], [x_], [x3d], x_)

                        def hf_cb(kc, hf):
                            k.op(pe, [(lambda tt=tt: nc.tensor.matmul(lg_ps[tt][:], lhsT=hf[:, tt * 128:(tt + 1) * 128], rhs=rt_s[:, kc, :],
                                                                      start=(kc == 0), stop=(kc == KC - 1))) for tt in range(4)], reads=[hf, rt_s], writes=lg_ps)
                        rmsnorm_tile(x_, nrm[1]["fn"], h2, s, npool, hf_cb=hf_cb)
                        k.dma(sp, h3_in.t[:, tsl].rearrange("(c p) t -> p c t", p=128), h2[:], [h2], [h3_in], h2)
                        k.op(dve, [(lambda tt=tt: nc.vector.tensor_copy(out=lgs[:, tt, :], in_=lg_ps[tt][:])) for tt in range(4)], reads=lg_ps, writes=[lgs], ss=True)
                        k.op(dve, [(lambda tt=tt: nc.vector.max(out=mx8[:, tt, :], in_=lgs[:, tt, :])) for tt in range(4)], reads=[lgs], writes=[mx8], ss=True)
                        k.op(dve, [(lambda tt=tt: nc.vector.tensor_scalar(out=msk[:, tt, :], in0=lgs[:, tt, :], scalar1=mx8[:, tt, 1:2], scalar2=None, op0=ALU.is_ge)) for tt in range(4)]
                             + [lambda: nc.vector.tensor_scalar(out=nv1[:], in0=mx8[:, :, 0], scalar1=-1.0, scalar2=None, op0=ALU.mult)], reads=[lgs, mx8], writes=[msk, nv1], ss=True)
                        k.op(act, [(lambda tt=tt: nc.scalar.activation(out=ee[:, tt, :], in_=lgs[:, tt, :], func=AF.Exp, bias=nv1[:, tt:tt + 1], scale=1.0)) for tt in range(4)],
                             reads=[lgs, nv1], writes=[ee], ss=True)
                        k.op(dve, lambda: nc.vector.tensor_tensor(out=ee[:], in0=ee[:], in1=msk[:], op=ALU.mult), reads=[ee, msk], writes=[ee], ss=True)
                        k.op(dve, lambda: nc.vector.tensor_reduce(out=den[:], in_=ee[:], axis=AX.X, op=ALU.add), reads=[ee], writes=[den], ss=True)
                        k.op(dve, lambda: nc.vector.reciprocal(out=den[:], in_=den[:]), reads=[den], writes=[den], ss=True)
                        k.op(dve, [(lambda tt=tt: nc.vector.tensor_scalar(out=ee[:, tt, :], in0=ee[:, tt, :], scalar1=den[:, tt:tt + 1], scalar2=None, op0=ALU.mult)) for tt in range(4)],
                             reads=[ee, den], writes=[ee], ss=True)
                        k.op(pe, [(lambda tt=tt: nc.tensor.matmul(gT_ps[:, tt * 128:(tt + 1) * 128], lhsT=ee[:, tt, :], rhs=c32[:, ident32, :], start=True, stop=True)) for tt in range(4)],
                             reads=[ee, c32], writes=[gT_ps])
                        k.op(dve, lambda: nc.vector.tensor_copy(out=gts[:], in_=gT_ps[:]), reads=[gT_ps], writes=[gts], ss=True)
                        k.dma(sp, gt_in.t[:, tsl], gts[:], [gts], [gt_in], gts)
            if i == 0:
                k.coll("AllGather", h_in[1], h_g[1])

        def phase_moe():
            k.coll("AllGather", h3_in, h3_g)
            k.coll("AllGather", gt_in, gt_g)
            NFC = DEXP // 128
            with k.scope() as s:
                hb = [k.sb(f"mh{j}", [128, KC, 512], BF16, s) for j in range(2)]
                actb = k.sb("mact", [128, NFC, 512], BF16, s)
                wpool = [k.sb(f"mw{j}", [128, max(NFC, 2 * KC), 256], BF16, s) for j in range(2)]
                pps = [k.ps(f"mp{j}", s) for j in range(6)]
                gps = k.ps("mgp", s)
                grow = [k.sb(f"mgr{j}", [1, TB], F32, s) for j in range(2)]
                gbc = [k.sb(f"mgb{j}", [128, 512], F32, s) for j in range(2)]
                sg = [k.sb(f"msg{j}", [128, 512], F32, s) for j in range(3)]
                ysb = [k.sb(f"my{j}", [128, 512], F32, s) for j in range(3)]
                wcnt, pcnt, ycnt = [0], [0], [0]
                gview = gt_g.t.rearrange("(r e) t -> e r t", e=NEXP)
                for tg in range(NQ):
                    r, t0 = divmod(tg * 512, TB)
                    h_ = hb[tg % 2]
                    gr, gb = grow[r % 2], gbc[tg % 2]
                    k.dma(sp, h_[:], h3_g.t[r * DM:(r + 1) * DM, t0:t0 + 512].rearrange("(c p) t -> p c t", p=128), [h3_g], [h_], h_)
                    if t0 == 0:
                        k.dma(sp, gr[:], gt_g.t[bass.ds(k.pid_sp + r * NEXP, 1), :], [gt_g], [gr], gr)
                    k.op(pe, lambda: nc.tensor.matmul(gps[:], lhsT=ones1[0:1, :], rhs=gr[:, t0:t0 + 512], start=True, stop=True), reads=[ones1, gr], writes=[gps])
                    k.op(act, lambda: nc.scalar.copy(out=gb[:], in_=gps[:]), reads=[gps], writes=[gb])
                    for g0 in range(0, DEXP, 256):
                        wb = wpool[wcnt[0] % len(wpool)]
                        wcnt[0] += 1
                        k.dma(sp, wb[:, 0:KC, :], wegb.t[:, g0:g0 + 256].rearrange("(c p) n -> p c n", p=128), [wegb], [wb], wb)
                        k.dma(sp, wb[:, KC:2 * KC, :], weub.t[:, g0:g0 + 256].rearrange("(c p) n -> p c n", p=128), [weub], [wb], wb)
                        for j in range(2):
                            fc = g0 // 128 + j
                            pg = pps[pcnt[0] % 6]
                            pu = pps[(pcnt[0] + 1) % 6]
                            pcnt[0] += 2
                            k.op(pe, [(lambda kc=kc, j=j, pg=pg, wb=wb: nc.tensor.matmul(pg[:], lhsT=wb[:, kc, j * 128:(j + 1) * 128], rhs=h_[:, kc, :],
                                                                                         start=(kc == 0), stop=(kc == KC - 1))) for kc in range(KC)],
                                 reads=[wb, h_], writes=[pg])
                            k.op(pe, [(lambda kc=kc, j=j, pu=pu, wb=wb: nc.tensor.matmul(pu[:], lhsT=wb[:, KC + kc, j * 128:(j + 1) * 128], rhs=h_[:, kc, :],
                                                                                         start=(kc == 0), stop=(kc == KC - 1))) for kc in range(KC)],
                                 reads=[wb, h_], writes=[pu])
                            sg_ = sg[fc % 3]
                            k.op(act, lambda pg=pg, sg_=sg_: nc.scalar.activation(out=sg_[:], in_=pg[:], func=AF.Silu), reads=[pg], writes=[sg_])
                            k.op(dve, lambda pu=pu, sg_=sg_: nc.vector.tensor_tensor(out=sg_[:], in0=pu[:], in1=sg_[:], op=ALU.mult), reads=[pu, sg_], writes=[sg_])
                            k.op(dve, lambda fc=fc, sg_=sg_: nc.vector.tensor_tensor(out=actb[:, fc, :], in0=sg_[:], in1=gb[:], op=ALU.mult), reads=[sg_, gb], writes=[actb])
                    for g0 in range(0, DM, 256):
                        wb = wpool[wcnt[0] % len(wpool)]
                        wcnt[0] += 1
                        for c0 in range(0, NFC, 14):
                            c1 = min(NFC, c0 + 14)
                            k.dma(sp, wb[:, c0:c1, :], wedb.t[c0 * 128:c1 * 128, g0:g0 + 256].rearrange("(c p) n -> p c n", p=128), [wedb], [wb], wb)
                        for j in range(2):
                            pb = pps[pcnt[0] % 6]
                            pcnt[0] += 1
                            oc = g0 // 128 + j
                            k.op(pe, [(lambda fc=fc, j=j, pb=pb, wb=wb: nc.tensor.matmul(pb[:], lhsT=wb[:, fc, j * 128:(j + 1) * 128], rhs=actb[:, fc, :],
                                                                                         start=(fc == 0), stop=(fc == NFC - 1))) for fc in range(NFC)],
                                 reads=[wb, actb], writes=[pb])
                            y_ = ysb[ycnt[0] % 3]
                            ycnt[0] += 1
                            k.op(act, lambda pb=pb, y_=y_: nc.scalar.copy(out=y_[:], in_=pb[:]), reads=[pb], writes=[y_])
                            k.dma(sp, y_in[r].t[oc * 128:(oc + 1) * 128, t0:t0 + 512], y_[:], [y_], [y_in[r]], y_)
                    if t0 + 512 == TB:
                        k.coll("AllReduce", y_in[r], y_rr[r])
                        k.dma(pool, y_rb[r].t, y_rr[r].t, [y_rr[r]], [y_rb[r]], y_rb[r])
            with k.scope() as s:
                xt = [k.sb(f"fx{j}", [128, KC, 512], F32, s) for j in range(2)]
                yt = [k.sb(f"fy{j}", [128, KC, 512], F32, s) for j in range(2)]
                yview = y_r.t.rearrange("(r c p) t -> p (r c) t", r=NCORES, p=128)
                for sbk in range(NSB):
                    x_, y_ = xt[sbk % 2], yt[sbk % 2]
                    tsl = slice(sbk * 512, (sbk + 1) * 512)
                    k.dma(sp, x_[:], x3d.t[:, tsl].rearrange("(c p) t -> p c t", p=128), [x3d], [x_], x_)
                    k.dma(sp, y_[:], yview[:, bass.ds(k.pid_sp * KC, KC), tsl], y_rb, [y_], y_)
                    k.op(dve, lambda: nc.vector.tensor_tensor(out=x_[:], in0=x_[:], in1=y_[:], op=ALU.add), reads=[x_, y_], writes=[x_])
                    k.dma(sp, outT.t[:, tsl].rearrange("(c p) t -> p c t", p=128), x_[:], [x_], [outT], x_)

        phase_norm0()
        gather_cast(wg0s, [256, D_FF], wg0f, wg0b, "wg0")
        gather_cast(wu0s, [256, D_FF], wu0f, wu0b, "wu0")
        gather_cast(wd0s, [D_FF // 8, DM], wd0f, wd0b, "wd0")
        gather_cast(L[1]["wout"], [256, DM], wout_f[1], wout_bf[1], "wo1")
        cast_rows(weg, wegb, DM)
        cast_rows(weu, weub, DM)
        cast_rows(wed, wedb, DEXP)
        for i in range(2):
            if stop != 1.5:
                lam_setup(i)
        for i in range(2):
            if stop >= 2 + 2 * i:
                if stop != 2.2:
                    phase_diff(i)
                if stop != 2.1:
                    phase_dil(i)
            if stop >= 3 + 2 * i:
                phase_tok(i)
        if stop >= 6:
            phase_moe()
        for nm, (b, o) in k.dbg_out.items():
            k.dma(pool, o.t, b.t, [b], [o], o)
        if k.dbg_out:
            for nm, (b, o) in k.dbg_out.items():
                pool.eng.wait_ge(o.ds.sem, o.ds.n)
        k.finish_all()
    return k


def _consts(S):
    pos = np.arange(S, dtype=np.float32)

    def tab(dim):
        inv = (np.float32(ROPE_THETA) ** (-np.arange(0, dim, 2, dtype=np.float32) / np.float32(dim))).astype(np.float32)
        ang = (pos[None, :] * inv[:, None]).astype(np.float32)
        return np.cos(ang).astype(np.float32), np.sin(ang).astype(np.float32)
    ca, sa = tab(64)
    cbb, sbb = tab(128)

    def rot(d):
        R = np.zeros((d, d), np.float32)
        for j in range(d // 2):
            R[j + d // 2, j] = -1.0
            R[j, j + d // 2] = 1.0
        return R
    cf32 = np.zeros((128, 3, 128), np.float32)
    cf32[0:64, 0, 0:64] = rot(64)
    cf32[64:128, 0, 64:128] = rot(64)
    cf32[:, 1, :] = rot(128)
    cf32[:, 2, :] = np.eye(128, dtype=np.float32)
    ii = np.arange(128)[:, None]
    jj = np.arange(128)[None, :]
    cbf = np.zeros((128, 5, 128), np.float32)
    cbf[:, 0, :] = (jj <= ii)
    cbf[:, 1, :] = (jj >= ii)
    cbf[:, 2, :] = (jj <= ii) & (ii >= 64)
    cbf[:, 3, :] = (jj >= ii) & (ii < 64)
    cbf[:, 4, :] = np.eye(128)
    return dict(cosa=ca, sina=sa, cosb=cbb, sinb=sbb, cf32=cf32, cbf=cbf.astype(ml_dtypes.bfloat16))


def make_in_maps(inputs, S):
    TB = S // NCORES
    cs = _consts(S)
    x = np.asarray(inputs["x"])[0]
    maps = []
    for c in range(NCORES):
        m = dict(cs)
        m["xT"] = np.ascontiguousarray(x[c * TB:(c + 1) * TB, :].T)
        for i in range(2):
            m[f"an{i}"] = np.ascontiguousarray(np.asarray(inputs[f"attn_norm_{i}"]).reshape(KC, 128).T)
            m[f"fn{i}"] = np.ascontiguousarray(np.asarray(inputs[f"ffn_norm_{i}"]).reshape(KC, 128).T)
            w_in = np.asarray(inputs[f"w_in_{i}"])
            cols = np.concatenate([np.arange(b * 1024 + c * 128, b * 1024 + (c + 1) * 128) for b in range(6)])
            m[f"win{i}"] = np.ascontiguousarray(w_in[:, cols])
            g = np.zeros((128, 6), np.float32)
            g[:, 0] = np.tile(np.asarray(inputs[f"diff_q_norm_{i}"]), 2)
            g[:, 1] = np.tile(np.asarray(inputs[f"diff_k_norm_{i}"]), 2)
            g[:, 2] = np.asarray(inputs[f"dil_q_norm_{i}"])
            g[:, 3] = np.asarray(inputs[f"dil_k_norm_{i}"])
            g[:, 4] = np.asarray(inputs[f"diff_out_norm_{i}"])
            g[:, 5] = np.asarray(inputs[f"dil_out_norm_{i}"])
            m[f"gains{i}"] = g
            m[f"lamv{i}"] = np.ascontiguousarray(np.stack([np.asarray(inputs[f"diff_lam_{n}_{i}"]) for n in ("q1", "k1", "q2", "k2")], axis=1))
            m[f"wout{i}"] = np.ascontiguousarray(np.asarray(inputs[f"w_out_{i}"])[c * 256:(c + 1) * 256])
        m["wg0"] = np.ascontiguousarray(np.asarray(inputs["ffn_w_gate_0"])[c * 256:(c + 1) * 256])
        m["wu0"] = np.ascontiguousarray(np.asarray(inputs["ffn_w_up_0"])[c * 256:(c + 1) * 256])
        m["wd0"] = np.ascontiguousarray(np.asarray(inputs["ffn_w_down_0"])[c * 704:(c + 1) * 704])
        m["router"] = np.ascontiguousarray(np.asarray(inputs["router_1"]).reshape(KC, 128, NEXP).transpose(1, 0, 2))
        m["weg"] = np.ascontiguousarray(np.asarray(inputs["moe_w_gate_1"])[c])
        m["weu"] = np.ascontiguousarray(np.asarray(inputs["moe_w_up_1"])[c])
        m["wed"] = np.ascontiguousarray(np.asarray(inputs["moe_w_down_1"])[c])
        maps.append(m)
    return maps


_CACHE = {}


def run(inputs, S, debug=False, stop=99, lite=False):
    key = (S, debug, stop, lite)
    if key not in _CACHE:
        _CACHE[key] = build(S, debug, stop, lite)
    kk = _CACHE[key]
    maps = make_in_maps(inputs, S)
    if lite:
        for m in maps:
            for nm in ("weg", "weu", "wed"):
                m[nm] = np.ascontiguousarray(m[nm][:256, :] if nm == "wed" else m[nm][:, :256])
    res = run_bass_kernel_spmd(kk.nc, maps, core_ids=list(range(NCORES)))
    out = np.concatenate([np.asarray(r["outT"]).T for r in res.results], axis=0)[None]
    return out.astype(np.float32), res


def kernel(**inputs):
    S = int(np.asarray(inputs["x"]).shape[1])
    out, _ = run(inputs, S)
    return out
```
